# Optimizing a Trainium2 kernel written in Bass

```python
import jax, jax.numpy as jnp
from jax import lax
import numpy as np

D_MODEL = 1024
BATCH = 8
SEQ = 4096
DEPTH = 1

N_MEM = 256
SC_WIDTH = D_MODEL
SC_KERNEL = 3
CF_WIDTH = D_MODEL
CF_KERNEL = 31
XA_HEADS = 4
XA_HEAD_DIM = D_MODEL // XA_HEADS
PEER_HEADS = 8
PEER_NKEYS = 128
PEER_TOPK = 16
PEER_QDIM = 256
PEER_HALF = PEER_QDIM // 2
PEER_EXPERTS = PEER_NKEYS * PEER_NKEYS
PEER_CHUNK = 128
IN_COLS = 3 * SC_WIDTH + 2 * CF_WIDTH + 2 * D_MODEL
EPS = 1e-6

kernel_name = 'hybrid_conv_memattn_peer_block'


def rms_norm(x, g):
    xf = x.astype(jnp.float32)
    y = xf * lax.rsqrt(jnp.mean(xf * xf, axis=-1, keepdims=True) + EPS)
    return (y * g.astype(jnp.float32)).astype(x.dtype)


def layer_norm(x, g, b):
    xf = x.astype(jnp.float32)
    mu = jnp.mean(xf, axis=-1, keepdims=True)
    var = jnp.mean(jnp.square(xf - mu), axis=-1, keepdims=True)
    y = (xf - mu) * lax.rsqrt(var + EPS)
    return (y * g.astype(jnp.float32) + b.astype(jnp.float32)).astype(x.dtype)


def causal_depthwise_conv(x, w):
    k, c = w.shape
    return lax.conv_general_dilated(
        x, w.reshape(k, 1, c).astype(x.dtype), window_strides=(1,), padding=[(k - 1, 0)],
        dimension_numbers=('NWC', 'WIO', 'NWC'), feature_group_count=c)


def conv_mixers(h, w_in, cf_b_pw1, sc_conv_w, sc_w_out, cf_conv_w, cf_conv_b, cf_ln_g, cf_ln_b,
                cf_w_pw2, cf_b_pw2, w_mix_out):
    proj = jnp.einsum('bsd,dc->bsc', h, w_in)
    cuts = [SC_WIDTH, 2 * SC_WIDTH, 3 * SC_WIDTH, 3 * SC_WIDTH + CF_WIDTH,
            3 * SC_WIDTH + 2 * CF_WIDTH, 3 * SC_WIDTH + 2 * CF_WIDTH + D_MODEL]
    sc_b, sc_c, sc_x, cf_a, cf_g, gate_sc, gate_cf = jnp.split(proj, cuts, axis=-1)
    y_sc = sc_b * causal_depthwise_conv(sc_c * sc_x, sc_conv_w)
    y_sc = jnp.einsum('bsc,cd->bsd', y_sc, sc_w_out)
    cf = (cf_a + cf_b_pw1[:CF_WIDTH]) * jax.nn.sigmoid(cf_g + cf_b_pw1[CF_WIDTH:])
    cf = causal_depthwise_conv(cf, cf_conv_w) + cf_conv_b
    cf = jax.nn.silu(layer_norm(cf, cf_ln_g, cf_ln_b))
    y_cf = jnp.einsum('bsc,cd->bsd', cf, cf_w_pw2) + cf_b_pw2
    merged = jax.nn.sigmoid(gate_sc) * y_sc + jax.nn.sigmoid(gate_cf) * y_cf
    return jnp.einsum('bsd,de->bse', merged, w_mix_out)


def memory_cross_attention(h, mem_n, w_q, w_kv, w_xo):
    b, s, _ = h.shape
    m = mem_n.shape[1]
    q = jnp.einsum('bsd,de->bse', h, w_q).reshape(b, s, XA_HEADS, XA_HEAD_DIM)
    kv = jnp.einsum('bmd,de->bme', mem_n, w_kv).reshape(b, m, 2, XA_HEADS, XA_HEAD_DIM)
    k, v = kv[:, :, 0], kv[:, :, 1]
    scores = jnp.einsum('bshd,bmhd->bhsm', q, k).astype(jnp.float32) * (XA_HEAD_DIM ** -0.5)
    probs = jax.nn.softmax(scores, axis=-1).astype(v.dtype)
    o = jnp.einsum('bhsm,bmhd->bshd', probs, v).reshape(b, s, XA_HEADS * XA_HEAD_DIM)
    return jnp.einsum('bse,ed->bsd', o, w_xo)


def peer_ffn(h, w_pq, subkeys, u, v):
    b, s, d = h.shape
    q = jnp.einsum('bsd,dq->bsq', h, w_pq).reshape(b, s, PEER_HEADS, 2, PEER_HALF)
    sub_scores = jnp.einsum('bshpc,pnc->bshpn', q, subkeys).astype(jnp.float32)
    top_s, top_i = lax.top_k(sub_scores, PEER_TOPK)
    n_cand = PEER_TOPK * PEER_TOPK
    cand_s = (top_s[..., 0, :, None] + top_s[..., 1, None, :]).reshape(b, s, PEER_HEADS, n_cand)
    cand_i = (top_i[..., 0, :, None] * PEER_NKEYS + top_i[..., 1, None, :]).reshape(b, s, PEER_HEADS, n_cand)
    fin_s, fin_pos = lax.top_k(cand_s, PEER_TOPK)
    expert_idx = jnp.take_along_axis(cand_i, fin_pos, axis=-1)
    gates = jax.nn.softmax(fin_s, axis=-1).astype(h.dtype)
    n_chunks = (b * s) // PEER_CHUNK
    h_c = h.reshape(n_chunks, PEER_CHUNK, d)
    e_c = expert_idx.reshape(n_chunks, PEER_CHUNK, PEER_HEADS, PEER_TOPK)
    g_c = gates.reshape(n_chunks, PEER_CHUNK, PEER_HEADS, PEER_TOPK)

    def chunk(args):
        hc, ec, gc = args
        u_sel = jnp.take(u, ec, axis=0)
        act = jax.nn.gelu(jnp.einsum('chkd,cd->chk', u_sel, hc), approximate=False) * gc
        v_sel = jnp.take(v, ec, axis=0)
        return jnp.einsum('chk,chkd->cd', act, v_sel)

    out = lax.map(chunk, (h_c, e_c, g_c))
    return out.reshape(b, s, d)


def setup_inputs(seed: int = 0) -> dict:
    key = jax.random.key(seed)
    ks = jax.random.split(key, 26)
    L = DEPTH

    def nrm(k, shape, scale):
        return jax.random.normal(k, shape, jnp.float32) * scale

    def gain(k, shape):
        return 1.0 + 0.02 * jax.random.normal(k, shape, jnp.float32)

    return {
        'x': nrm(ks[0], (BATCH, SEQ, D_MODEL), 1.0),
        'mem': nrm(ks[1], (BATCH, N_MEM, D_MODEL), 1.0),
        'norm_mix_g': gain(ks[2], (L, D_MODEL)),
        'w_in': nrm(ks[3], (L, D_MODEL, IN_COLS), D_MODEL ** -0.5),
        'cf_b_pw1': nrm(ks[4], (L, 2 * CF_WIDTH), 0.02),
        'sc_conv_w': nrm(ks[5], (L, SC_KERNEL, SC_WIDTH), SC_KERNEL ** -0.5),
        'sc_w_out': nrm(ks[6], (L, SC_WIDTH, D_MODEL), SC_WIDTH ** -0.5),
        'cf_conv_w': nrm(ks[7], (L, CF_KERNEL, CF_WIDTH), CF_KERNEL ** -0.5),
        'cf_conv_b': nrm(ks[8], (L, CF_WIDTH), 0.02),
        'cf_ln_g': gain(ks[9], (L, CF_WIDTH)),
        'cf_ln_b': nrm(ks[10], (L, CF_WIDTH), 0.02),
        'cf_w_pw2': nrm(ks[11], (L, CF_WIDTH, D_MODEL), CF_WIDTH ** -0.5),
        'cf_b_pw2': nrm(ks[12], (L, D_MODEL), 0.02),
        'w_mix_out': nrm(ks[13], (L, D_MODEL, D_MODEL), D_MODEL ** -0.5),
        'norm_xa_g': gain(ks[14], (L, D_MODEL)),
        'norm_mem_g': gain(ks[15], (L, D_MODEL)),
        'w_q': nrm(ks[16], (L, D_MODEL, XA_HEADS * XA_HEAD_DIM), D_MODEL ** -0.5),
        'w_kv': nrm(ks[17], (L, D_MODEL, 2 * XA_HEADS * XA_HEAD_DIM), D_MODEL ** -0.5),
        'w_xo': nrm(ks[18], (L, XA_HEADS * XA_HEAD_DIM, D_MODEL), D_MODEL ** -0.5),
        'norm_peer_g': gain(ks[19], (L, D_MODEL)),
        'w_peer_q': nrm(ks[20], (L, D_MODEL, PEER_HEADS * PEER_QDIM), D_MODEL ** -0.5),
        'peer_subkeys': nrm(ks[21], (L, 2, PEER_NKEYS, PEER_HALF), PEER_HALF ** -0.5),
        'peer_u': nrm(ks[22], (L, PEER_EXPERTS, D_MODEL), D_MODEL ** -0.5),
        'peer_v': nrm(ks[23], (L, PEER_EXPERTS, D_MODEL), PEER_HEADS ** -0.5),
        'final_norm_g': gain(ks[24], (D_MODEL,)),
    }


def reference(x, mem, norm_mix_g, w_in, cf_b_pw1, sc_conv_w, sc_w_out, cf_conv_w, cf_conv_b, cf_ln_g,
              cf_ln_b, cf_w_pw2, cf_b_pw2, w_mix_out, norm_xa_g, norm_mem_g, w_q, w_kv, w_xo, norm_peer_g,
              w_peer_q, peer_subkeys, peer_u, peer_v, final_norm_g):
    for l in range(DEPTH):
        x = x + conv_mixers(rms_norm(x, norm_mix_g[l]), w_in[l], cf_b_pw1[l], sc_conv_w[l], sc_w_out[l],
                            cf_conv_w[l], cf_conv_b[l], cf_ln_g[l], cf_ln_b[l], cf_w_pw2[l], cf_b_pw2[l],
                            w_mix_out[l])
        mem_n = rms_norm(mem, norm_mem_g[l])
        x = x + memory_cross_attention(rms_norm(x, norm_xa_g[l]), mem_n, w_q[l], w_kv[l], w_xo[l])
        x = x + peer_ffn(rms_norm(x, norm_peer_g[l]), w_peer_q[l], peer_subkeys[l], peer_u[l], peer_v[l])
    return rms_norm(x, final_norm_g)
```

```python
import contextlib
import numpy as np
import concourse.bass as bass
import concourse.mybir as mybir
from concourse.bass_utils import run_bass_kernel_spmd

F32 = mybir.dt.float32
BF16 = mybir.dt.bfloat16
U32 = mybir.dt.uint32
AF = mybir.ActivationFunctionType
ALU = mybir.AluOpType
AX = mybir.AxisListType

D = 1024
SEQ = 4096
NB_TOK = 256
NBLK = SEQ // NB_TOK
NMEM = 256
EPS = 1e-6
UNIT = 8192
U_WIN = 0
U_SCO, U_PW2, U_MIX, U_WQ, U_WXO, U_PQ0, U_PQ1, U_WK, U_WV = 8, 9, 10, 11, 12, 13, 14, 15, 16
U_PEER = 17
NUNITS = U_PEER + 32
PC_GMIX, PC_B1A, PC_B1G, PC_CONVB, PC_LNG, PC_LNB, PC_BPW2, PC_GXA, PC_GMEM, PC_GPEER, PC_GFIN = [8 * i for i in range(11)]
PC_SCW = 88
PC_CFW = PC_SCW + 24
PC_SKT = PC_CFW + 248
PC_IDENT = PC_SKT + 256
PC_IOTA = PC_IDENT + 128
NPAR = PC_IOTA + 128

SAME_ENG_SYNC = True


class Sched:
    ENG = ('pe', 'act', 'dve', 'pool', 'sp')

    def __init__(self, nc, es, nslots=24):
        self.nc = nc
        self.eng = {'pe': nc.tensor, 'act': nc.scalar, 'dve': nc.vector, 'pool': nc.gpsimd, 'sp': nc.sync}
        self.sem = {e: es.enter_context(nc.semaphore('sem_' + e)) for e in self.ENG}
        self.cnt = {e: 0 for e in self.ENG}
        self.dsem = [es.enter_context(nc.semaphore('dsem%d' % i)) for i in range(nslots)]
        self.dcnt = [0] * nslots
        self.dnext = 0
        self.seen = {e: {} for e in self.ENG}
        self.lastw = {}
        self.readers = {}
        self.nwaits = 0
        self.rkeys = set()
        self.prevR = {}
        self.curR = {}

    def phase(self, names):
        for k, v in self.curR.items():
            if self.prevR.get(k, 0) < v:
                self.prevR[k] = v
        self.curR = {}
        self.rkeys = set(names)

    def _touchR(self, reads, writes):
        for k in list(reads) + list(writes):
            nm = k if isinstance(k, str) else k[0]
            if nm in self.rkeys:
                return True
        return False

    def _semof(self, src):
        return self.dsem[src[1]] if isinstance(src, tuple) else self.sem[src]

    @staticmethod
    def _isps(k):
        return isinstance(k, tuple) and k[0] == 'psb'

    def _wait(self, e, deps, embed=False):
        need = {}
        for (src, val, isps) in deps:
            if src == e and (e == 'pe' or isps or not SAME_ENG_SYNC):
                continue
            if need.get(src, 0) < val:
                need[src] = val
        pend = [(src, val) for src, val in need.items() if self.seen[e].get(src, 0) < val]
        emb = pend.pop() if (embed and pend) else None
        for src, val in pend:
            self.eng[e].wait_ge(self._semof(src), val)
            self.nwaits += 1
        for src, val in pend + ([emb] if emb else []):
            self.seen[e][src] = val
        return emb

    def _deps(self, reads, writes):
        deps = []
        for k in reads:
            if self._isps(k):
                continue
            if k in self.lastw:
                deps.append(self.lastw[k] + (False,))
        for k in list(writes) + [k for k in reads if self._isps(k)]:
            ps = self._isps(k)
            if k in self.lastw:
                deps.append(self.lastw[k] + (ps,))
            r = self.readers.get(k)
            if r:
                deps.extend((a, b, ps) for a, b in r.items())
        return deps

    def _record(self, tag, reads, writes):
        writes = list(writes) + [k for k in reads if self._isps(k)]
        reads = [k for k in reads if not self._isps(k)]
        for k in writes:
            if self._isps(k) and k in self.lastw:
                r = self.readers.setdefault(k, {})
                lw = self.lastw[k]
                if lw[0] != tag[0] and r.get(lw[0], 0) < lw[1]:
                    r[lw[0]] = lw[1]
                r.pop(tag[0], None)
                self.lastw[k] = tag
            else:
                self.lastw[k] = tag
                self.readers[k] = {}
        for k in reads:
            r = self.readers.setdefault(k, {})
            if r.get(tag[0], 0) < tag[1]:
                r[tag[0]] = tag[1]

    def op(self, e, fn, reads=(), writes=(), inc=True, embed=True):
        deps = self._deps(reads, writes)
        tr = self._touchR(reads, writes)
        if tr:
            deps.extend((a, b, False) for a, b in self.prevR.items())
        emb = self._wait(e, deps, embed=(embed and e in ('act', 'dve')))
        ins = fn()
        if emb is not None:
            ins._wait_ge(self._semof(emb[0]), emb[1])
        if inc:
            self.cnt[e] += 1
            ins.then_inc(self.sem[e], 1)
            tag = (e, self.cnt[e])
        else:
            tag = (e, self.cnt[e] + 1)
        self._record(tag, reads, writes)
        if tr and self.curR.get(tag[0], 0) < tag[1]:
            self.curR[tag[0]] = tag[1]
        return ins

    def dma(self, q, out, in_, reads=(), writes=()):
        slot = self.dnext
        self.dnext = (self.dnext + 1) % len(self.dsem)
        deps = self._deps(reads, writes)
        tr = self._touchR(reads, writes)
        if tr:
            deps.extend((a, b, False) for a, b in self.prevR.items())
        if self.dcnt[slot] > 0:
            deps.append((('d', slot), self.dcnt[slot], False))
        self._wait(q, deps)
        ins = self.eng[q].dma_start(out=out, in_=in_)
        self.dcnt[slot] += 16
        ins.then_inc(self.dsem[slot], 16)
        tag = (('d', slot), self.dcnt[slot])
        self._record(tag, reads, writes)
        if tr:
            self.curR[tag[0]] = tag[1]
        return ins

    def barrier(self, engines=None):
        deps = [(e, self.cnt[e], False) for e in self.ENG if self.cnt[e] > 0]
        deps += [(('d', i), c, False) for i, c in enumerate(self.dcnt) if c > 0]
        for e in (engines or self.ENG):
            self._wait(e, [d for d in deps if d[0] != e])


class Mem:
    def __init__(self, big, nbytes):
        self.big = big
        self.n = nbytes
        self.top = 0

    def alloc(self, shape, dtype):
        esz = 2 if dtype == BF16 else 4
        n = int(np.prod(shape[1:])) * esz
        off = (self.top + 63) // 64 * 64
        assert off + n <= self.n, ("SBUF region overflow", off + n, self.n)
        self.top = off + n
        v = self.big[:, off // 4:(off + n) // 4]
        if dtype != F32:
            v = v.bitcast(dtype)
        if len(shape) > 2:
            names = 'abcd'[:len(shape) - 1]
            pat = 'p (%s) -> p %s' % (' '.join(names), ' '.join(names))
            v = v.rearrange(pat, **{nm: s for nm, s in zip(names, shape[1:])})
        return v


def build_nc(nblk=NBLK, phases=('mixer', 'attn', 'peer'), prepass=True, stop_at=None, nunits_pp=NUNITS):
    nc = bass.Bass("TRN2", target_bir_lowering=False)
    T = NB_TOK
    xr = nc.dram_tensor("xr", [NBLK, 128, 8 * T], F32, kind="ExternalInput").ap()
    memr = nc.dram_tensor("memr", [128, 8 * NMEM], F32, kind="ExternalInput").ap()
    wsrc = nc.dram_tensor("wunits", [NUNITS, 128, UNIT], F32, kind="ExternalInput").ap()
    parr = nc.dram_tensor("params", [128, NPAR], F32, kind="ExternalInput").ap()
    outr = nc.dram_tensor("outr", [NBLK, 128, 8 * T], F32, kind="ExternalOutput").ap()
    wscr = nc.dram_tensor("wscr", [NUNITS, 128, UNIT], BF16, kind="Internal").ap()

    es = contextlib.ExitStack()
    SB_BYTES = 212800
    big = es.enter_context(nc.sbuf_tensor("big", [128, SB_BYTES // 4], F32))
    psb = [es.enter_context(nc.psum_tensor("ps%d" % i, [128, 512], F32)) for i in range(8)]
    S = Sched(nc, es)
    M = Mem(big, SB_BYTES)

    par = M.alloc([128, NPAR], F32)
    xT = M.alloc([128, 8, T], F32)
    xT_b = M.alloc([128, 8, T], F32)
    hT = M.alloc([128, 8, T], BF16)
    hT2 = M.alloc([128, 8, T], BF16)
    kT = M.alloc([128, 8, NMEM], BF16)
    vv = M.alloc([128, 2, D], BF16)
    kmax = M.alloc([128, 8], F32)
    maskt = M.alloc([128, 16], U32)
    iotau = M.alloc([128, 128], U32)
    onesb = M.alloc([128, 128], BF16)
    skTb = M.alloc([128, 2, 128], BF16)
    prodbuf = M.alloc([128, 8, T + 2], BF16)
    glubuf = M.alloc([128, 8, T + 30], BF16)
    sqb = M.alloc([128, 8, T], BF16)
    rs0 = M.alloc([128, T], F32)
    rs1 = M.alloc([128, T], F32)
    ring = M.alloc([128, 4, UNIT], BF16)
    PTOP = M.top

    ident = par[:, PC_IDENT:PC_IDENT + 128]
    iota = par[:, PC_IOTA:PC_IOTA + 128]

    def pcol(base, k, n=1):
        return par[:, base + k:base + k + n]

    def pst(i):
        return psb[i // 2][:, (i % 2) * 256:(i % 2) * 256 + 256]

    def psk(i):
        return ('psb', i // 2)

    class Rot:
        def __init__(self, ids):
            self.ids = sorted(ids, key=lambda i: (i % 2, i // 2))
            self.i = 0

        def next(self):
            v = self.ids[self.i % len(self.ids)]
            self.i += 1
            return v

    ring_state = {'n': 0}

    def ring_load(unit):
        slot = ring_state['n'] % 4
        ring_state['n'] += 1
        S.dma('sp', out=ring[:, slot, :], in_=wscr[unit], reads=[('wscr', unit)], writes=[('ring', slot)])
        return slot

    def mm(out, lhsT, rhs, start, stop, reads, wkey, inc):
        return S.op('pe', lambda: nc.tensor.matmul(out, lhsT=lhsT, rhs=rhs, start=start, stop=stop),
                    reads=reads, writes=[wkey], inc=inc)

    def act(out, in_, func, reads, writes, bias=None, scale=None):
        kw = {}
        if bias is not None:
            kw['bias'] = bias
        if scale is not None:
            kw['scale'] = scale
        return S.op('act', lambda: nc.scalar.activation(out=out, in_=in_, func=func, **kw), reads=reads, writes=writes)

    def tt(e, out, in0, in1, op, reads, writes):
        return S.op(e, lambda: S.eng[e].tensor_tensor(out=out, in0=in0, in1=in1, op=op), reads=reads, writes=writes)

    def ts(e, out, in0, s1, s2, op0, op1, reads, writes):
        if op1 is None:
            return S.op(e, lambda: S.eng[e].tensor_scalar(out=out, in0=in0, scalar1=s1, scalar2=None, op0=op0),
                        reads=reads, writes=writes)
        return S.op(e, lambda: S.eng[e].tensor_scalar(out=out, in0=in0, scalar1=s1, scalar2=s2, op0=op0, op1=op1),
                    reads=reads, writes=writes)

    def stt(e, out, in0, scalar, in1, op0, op1, reads, writes):
        return S.op(e, lambda: S.eng[e].scalar_tensor_tensor(out=out, in0=in0, scalar=scalar, in1=in1, op0=op0, op1=op1),
                    reads=reads, writes=writes)

    def cp(e, out, in_, reads, writes):
        if e == 'act':
            return S.op('act', lambda: nc.scalar.copy(out=out, in_=in_), reads=reads, writes=writes)
        return S.op(e, lambda: S.eng[e].tensor_copy(out=out, in_=in_), reads=reads, writes=writes)

    def rmsnorm_to(src, srckeys, gbase, dst, dstkeys, ncols, pt, out_dtype_is_bf16=True):
        for c in range(8):
            act(sqb[:, c, 0:ncols], src[:, c, :], AF.Square, reads=[srckeys[c]], writes=[('sqb', c)])
        for c in range(8):
            mm(pst(pt)[:, 0:ncols], onesb[:, :], sqb[:, c, 0:ncols], c == 0, c == 7,
               reads=[('sqb', c), 'onesb'], wkey=psk(pt), inc=(c == 7))
        act(rs0[:, 0:ncols], pst(pt)[:, 0:ncols], AF.Sqrt, reads=[psk(pt)], writes=['rs0'], bias=EPS, scale=1.0 / D)
        S.op('dve', lambda: nc.vector.reciprocal(out=rs1[:, 0:ncols], in_=rs0[:, 0:ncols]), reads=['rs0'], writes=['rs1'])
        for c in range(8):
            stt('dve', dst[:, c, :], src[:, c, :], pcol(gbase, c), rs1[:, 0:ncols], ALU.mult, ALU.mult,
                reads=[srckeys[c], 'rs1', 'par'], writes=[dstkeys[c]])

    S.dma('sp', out=par[:, :], in_=parr[:, :], writes=['par'])
    S.op('pool', lambda: nc.gpsimd.memset(onesb[:, :], 1.0), writes=['onesb'])
    S.op('dve', lambda: nc.vector.memset(maskt[:, 0:8], 0xFFFFC000), writes=['maskt'])
    S.op('dve', lambda: nc.vector.memset(maskt[:, 8:16], 0xFFFFFF80), writes=['maskt'])
    cp('dve', iotau[:, :], iota, reads=['par', 'maskt'], writes=['iotau'])
    cp('dve', skTb[:, :, :], par[:, PC_SKT:PC_SKT + 256].rearrange('p (a b) -> p a b', a=2), reads=['par'], writes=['skTb'])
    S.op('pool', lambda: nc.gpsimd.memset(prodbuf[:, :, :], 0.0), writes=['prodbuf'])
    S.op('pool', lambda: nc.gpsimd.memset(glubuf[:, :, :], 0.0), writes=['glubuf'])
    if stop_at == 'consts':
        S.barrier(); es.close(); return nc, S
    if prepass:
        M.top = PTOP
        S.phase(['stage'])
        NST = 2
        stage = M.alloc([128, NST, UNIT], F32)
        cuts = [0, 2944, 5888, UNIT]
        cengs = ['act', 'dve', 'pool']
        order = [U_WK, U_WV] + [u for u in range(nunits_pp) if u not in (U_WK, U_WV)]
        order = [u for u in order if u < nunits_pp or u in (U_WK, U_WV)]

        def pp_load(i):
            S.dma('sp', out=stage[:, i % NST, :], in_=wsrc[order[i]], writes=[('stage', i % NST)])
        for i in range(min(NST, len(order))):
            pp_load(i)
        for i, u in enumerate(order):
            sb = i % NST
            slot = i % 4
            for pi in range(3):
                a, b = cuts[pi], cuts[pi + 1]
                cp(cengs[pi], ring[:, slot, a:b], stage[:, sb, a:b], reads=[('stage', sb)], writes=[('ring', slot)])
            S.dma('sp', out=wscr[u], in_=ring[:, slot, :], reads=[('ring', slot)], writes=[('wscr', u)])
            if i + NST < len(order):
                pp_load(i + NST)
        ring_state['n'] = len(order) % 4
    if stop_at == 'prepass':
        S.barrier(); es.close(); return nc, S
    M.top = PTOP
    S.phase(['mT', 'absk'])
    mT = M.alloc([128, 8, NMEM], F32)
    absk = M.alloc([128, NMEM], F32)
    S.dma('sp', out=mT.rearrange('p a b -> p (a b)'), in_=memr[:, :], writes=[('mT', c) for c in range(8)])
    sK = ring_load(U_WK)
    sV = ring_load(U_WV)
    rmsnorm_to(mT, [('mT', c) for c in range(8)], PC_GMEM, hT, [('hT', c) for c in range(8)], NMEM, 0)
    if stop_at == 'kv1':
        S.barrier(); es.close(); return nc, S
    rK = ring[:, sK, :].rearrange('p (a b) -> p a b', a=8)
    rV = ring[:, sV, :].rearrange('p (a b) -> p a b', a=8)
    hkeys = [('hT', c) for c in range(8)]
    rot = Rot(range(2, 12))
    for e in range(8):
        pt = rot.next()
        for dc in range(8):
            mm(pst(pt), rK[:, dc, e * 128:(e + 1) * 128], hT[:, dc, :], dc == 0, dc == 7,
               reads=[hkeys[dc], ('ring', sK)], wkey=psk(pt), inc=(dc == 7))
        import os
        SK = os.environ.get('KV_SKIP', '')
        if 'act' not in SK:
            cp('act', kT[:, e, :], pst(pt), reads=[psk(pt)], writes=[('kT', e)])
        if 'dve' not in SK:
            stt('dve', absk[:, :], kT[:, e, :], -1.0, kT[:, e, :], ALU.mult, ALU.max, reads=[('kT', e)], writes=['absk'])
        if 'red' not in SK:
            S.op('dve', lambda e=e: nc.vector.reduce_max(out=kmax[:, e:e + 1], in_=absk[:, :], axis=AX.X),
                 reads=['absk'], writes=['kmax'])
    if stop_at == 'kv2':
        S.barrier(); es.close(); return nc, S
    for mc in range(2):
        for eh in range(2):
            bank = 6 + (mc * 2 + eh) % 2
            for dc in range(8):
                mm(psb[bank][:, :], hT[:, dc, mc * 128:(mc + 1) * 128], rV[:, dc, eh * 512:(eh + 1) * 512], dc == 0, dc == 7,
                   reads=[hkeys[dc], ('ring', sV)], wkey=('psb', bank), inc=(dc == 7))
            cp('act', vv[:, mc, eh * 512:(eh + 1) * 512], psb[bank][:, :], reads=[('psb', bank)], writes=['vv'])

    if stop_at == 'kv':
        S.barrier(); es.close(); return nc, S

    T_ = T
    xTs = [xT, xT_b]
    M.top = PTOP
    LISTS = M.alloc([128, 3, T], BF16)
    Ism, Jsm, Gsm = LISTS[:, 0, :], LISTS[:, 1, :], LISTS[:, 2, :]
    SS_OFF = M.top
    ssb = M.alloc([128, 2, 16, 128], F32)
    DYN = M.top

    def xk(b):
        return [('xT', b % 2, c) for c in range(8)]
    hkeys2 = [('hT2', c) for c in range(8)]

    def load_x(b):
        S.dma('sp', out=xTs[b % 2].rearrange('p a b -> p (a b)'), in_=xr[b], writes=xk(b))

    MIX_NAMES = ['ysc', 'zz', 'gsc', 'gcf', 'cfa', 'mg', 'scc', 'scb', 'sg', 'ycf', 'm1', 'm2', 'zb', 'z2b', 'dcf', 'dsc1',
                 'mean', 'msq', 'var', 'lrs']
    TK_NAMES = ['ss', 'tv', 'ti', 'tif', 'cand', 'code', 'tish', 'fv', 'fpos', 'fa', 'fb', 'faf', 'fbf', 'oh', 'If', 'Jf', 'Gf', 'dsm', 'esm',
                'zs', 'rz']

    def gen_mixer(b, base):
        xb = xTs[b % 2]
        xkeys = xk(b)
        M.top = base
        ysc = M.alloc([128, 8, T], BF16)
        zz = M.alloc([128, 8, T], F32)
        gsc = M.alloc([128, 8, T], BF16)
        gcf = M.alloc([128, 8, T], BF16)
        cfa = M.alloc([128, 8, T], BF16)
        merged = M.alloc([128, 8, T], BF16)
        scc = M.alloc([128, T], F32)
        scb = M.alloc([128, T], F32)
        sg = M.alloc([128, T], F32)
        ycf = M.alloc([128, T], F32)
        m1 = M.alloc([128, T], F32)
        zb = M.alloc([128, 2, T], BF16)
        z2b = M.alloc([128, 2, T], BF16)
        dcf = M.alloc([128, 1, 31, 128], BF16)
        dsc1 = M.alloc([128, 2, 3, 128], BF16)
        mean = M.alloc([128, T], F32)
        msq = M.alloc([128, T], F32)
        var = M.alloc([128, T], F32)
        lrs = M.alloc([128, T], F32)
        if b > 0:
            cp('pool', prodbuf[:, :, 0:2], prodbuf[:, :, T:T + 2], reads=['prodbuf'], writes=['prodbuf'])
            cp('pool', glubuf[:, :, 0:30], glubuf[:, :, T:T + 30], reads=['glubuf'], writes=['glubuf'])
        rmsnorm_to(xb, xkeys, PC_GMIX, hT2, hkeys2, T, 0)
        yield
        rot = Rot(range(2, 12))
        PS_SUM, PS_SQ = 14, 1
        slots = {}
        slots[0] = ring_load(U_WIN + 0)
        slots[1] = ring_load(U_WIN + 1)

        def stats(kk):
            d2 = kk % 2
            mm(pst(PS_SUM), onesb[:, :], zb[:, d2, :], kk == 0, kk == 7, reads=[('zb', d2), 'onesb'], wkey=psk(PS_SUM), inc=True)
            mm(pst(PS_SQ), onesb[:, :], z2b[:, d2, :], kk == 0, kk == 7, reads=[('z2b', d2), 'onesb'], wkey=psk(PS_SQ), inc=True)
        for k in range(8):
            if k + 2 < 8:
                slots[k + 2] = ring_load(U_WIN + k + 2)
            sl = slots[k]
            rw = ring[:, sl, 0:7168].rearrange('p (a f c) -> p a f c', a=8, f=7)
            db = k % 2
            tt('pool', dcf[:, 0, :, :], ident.unsqueeze(1).broadcast_to([128, 31, 128]),
               par[:, PC_CFW + 31 * k:PC_CFW + 31 * k + 31].unsqueeze(2).broadcast_to([128, 31, 128]), ALU.mult,
               reads=['par'], writes=[('dcf', 0)])
            tt('pool', dsc1[:, db, :, :], ident.unsqueeze(1).broadcast_to([128, 3, 128]),
               par[:, PC_SCW + 3 * k:PC_SCW + 3 * k + 3].unsqueeze(2).broadcast_to([128, 3, 128]), ALU.mult,
               reads=['par'], writes=[('dsc1', db)])

            def proj(fam):
                pt = rot.next()
                for dc in range(8):
                    mm(pst(pt), rw[:, dc, fam, :], hT2[:, dc, :], dc == 0, dc == 7,
                       reads=[hkeys2[dc], ('ring', sl)], wkey=psk(pt), inc=(dc == 7))
                return pt
            p_c = proj(1)
            cp('act', scc[:, :], pst(p_c), reads=[psk(p_c)], writes=['scc'])
            p_x = proj(2)
            tt('dve', prodbuf[:, k, 2:T + 2], pst(p_x), scc[:, :], ALU.mult, reads=[psk(p_x), 'scc'], writes=['prodbuf'])
            yield
            p_g = proj(4)
            act(sg[:, :], pst(p_g), AF.Sigmoid, reads=[psk(p_g), 'par'], writes=['sg'], bias=pcol(PC_B1G, k))
            p_a = proj(3)
            stt('dve', glubuf[:, k, 30:T + 30], pst(p_a), pcol(PC_B1A, k), sg[:, :], ALU.add, ALU.mult,
                reads=[psk(p_a), 'sg', 'par'], writes=['glubuf'])
            yield
            p_b = proj(0)
            cp('act', scb[:, :], pst(p_b), reads=[psk(p_b)], writes=['scb'])
            p_cv = rot.next()
            for tap in range(3):
                mm(pst(p_cv), dsc1[:, db, tap, :], prodbuf[:, k, tap:tap + T], tap == 0, tap == 2,
                   reads=['prodbuf', ('dsc1', db)], wkey=psk(p_cv), inc=(tap == 2))
            tt('dve', ysc[:, k, :], pst(p_cv), scb[:, :], ALU.mult, reads=[psk(p_cv), 'scb'], writes=[('ysc', k)])
            yield
            p_1 = proj(5)
            act(gsc[:, k, :], pst(p_1), AF.Sigmoid, reads=[psk(p_1)], writes=[('gsc', k)])
            p_2 = proj(6)
            act(gcf[:, k, :], pst(p_2), AF.Sigmoid, reads=[psk(p_2)], writes=[('gcf', k)])
            yield
            p_cv = rot.next()
            for tap in range(31):
                mm(pst(p_cv), dcf[:, 0, tap, :], glubuf[:, k, tap:tap + T], tap == 0, tap == 30,
                   reads=['glubuf', ('dcf', 0)], wkey=psk(p_cv), inc=(tap == 30))
            act(zz[:, k, :], pst(p_cv), AF.Identity, reads=[psk(p_cv), 'par'], writes=[('zz', k)], bias=pcol(PC_CONVB, k))
            act(z2b[:, db, :], pst(p_cv), AF.Square, reads=[psk(p_cv), 'par'], writes=[('z2b', db)], bias=pcol(PC_CONVB, k))
            cp('dve', zb[:, db, :], zz[:, k, :], reads=[('zz', k)], writes=[('zb', db)])
            if k > 0:
                stats(k - 1)
            yield
        stats(7)
        s_sco = ring_load(U_SCO)
        s_pw2 = ring_load(U_PW2)
        ts('dve', mean[:, :], pst(PS_SUM), 1.0 / D, None, ALU.mult, None, reads=[psk(PS_SUM)], writes=['mean'])
        tt('dve', msq[:, :], mean[:, :], mean[:, :], ALU.mult, reads=['mean'], writes=['msq'])
        stt('dve', var[:, :], pst(PS_SQ), 1.0 / D, msq[:, :], ALU.mult, ALU.subtract, reads=[psk(PS_SQ), 'msq'], writes=['var'])
        act(msq[:, :], var[:, :], AF.Sqrt, reads=['var'], writes=['msq'], bias=EPS, scale=1.0)
        S.op('dve', lambda: nc.vector.reciprocal(out=lrs[:, :], in_=msq[:, :]), reads=['msq'], writes=['lrs'])
        yield
        for k in range(8):
            tt('dve', zz[:, k, :], zz[:, k, :], mean[:, :], ALU.subtract, reads=[('zz', k), 'mean'], writes=[('zz', k)])
            tt('dve', zz[:, k, :], zz[:, k, :], lrs[:, :], ALU.mult, reads=[('zz', k), 'lrs'], writes=[('zz', k)])
            act(cfa[:, k, :], zz[:, k, :], AF.Silu, reads=[('zz', k), 'par'], writes=[('cfa', k)],
                bias=pcol(PC_LNB, k), scale=pcol(PC_LNG, k))
            if k % 2 == 1:
                yield
        r_sco = ring[:, s_sco, :].rearrange('p (a b) -> p a b', a=8)
        r_pw2 = ring[:, s_pw2, :].rearrange('p (a b) -> p a b', a=8)
        s_mix = ring_load(U_MIX)
        for e in range(8):
            pa = rot.next()
            for c in range(8):
                mm(pst(pa), r_sco[:, c, e * 128:(e + 1) * 128], ysc[:, c, :], c == 0, c == 7,
                   reads=[('ysc', c), ('ring', s_sco)], wkey=psk(pa), inc=(c == 7))
            pb = rot.next()
            for c in range(8):
                mm(pst(pb), r_pw2[:, c, e * 128:(e + 1) * 128], cfa[:, c, :], c == 0, c == 7,
                   reads=[('cfa', c), ('ring', s_pw2)], wkey=psk(pb), inc=(c == 7))
            act(ycf[:, :], pst(pb), AF.Identity, reads=[psk(pb), 'par'], writes=['ycf'], bias=pcol(PC_BPW2, e))
            tt('dve', m1[:, :], pst(pa), gsc[:, e, :], ALU.mult, reads=[psk(pa), ('gsc', e)], writes=['m1'])
            tt('dve', ycf[:, :], ycf[:, :], gcf[:, e, :], ALU.mult, reads=['ycf', ('gcf', e)], writes=['ycf'])
            tt('dve', merged[:, e, :], m1[:, :], ycf[:, :], ALU.add, reads=['m1', 'ycf'], writes=[('mg', e)])
            yield
        r_mix = ring[:, s_mix, :].rearrange('p (a b) -> p a b', a=8)
        for e in range(8):
            pa = rot.next()
            for c in range(8):
                mm(pst(pa), r_mix[:, c, e * 128:(e + 1) * 128], merged[:, c, :], c == 0, c == 7,
                   reads=[('mg', c), ('ring', s_mix)], wkey=psk(pa), inc=(c == 7))
            tt('dve', xb[:, e, :], xb[:, e, :], pst(pa), ALU.add, reads=[xkeys[e], psk(pa)], writes=[xkeys[e]])
            if e % 2 == 1:
                yield

    def attn(b):
        xb = xTs[b % 2]
        xkeys = xk(b)
        M.top = DYN
        S.phase(['qT', 'oT', 'aq', 'bnd', 'dd', 'eT', 'rinv', 'yo'])
        qT = M.alloc([128, 8, T], BF16)
        oT = M.alloc([128, 8, T], BF16)
        aq = M.alloc([128, 8, T], BF16)
        bnd = M.alloc([128, 4, T], F32)
        dd = M.alloc([128, 8, T], F32)
        eT = M.alloc([128, 8, T], BF16)
        rinv = M.alloc([128, 4, T], F32)
        s_q = ring_load(U_WQ)
        s_xo = ring_load(U_WXO)
        rmsnorm_to(xb, xkeys, PC_GXA, hT2, hkeys2, T, 0)
        r_q = ring[:, s_q, :].rearrange('p (a b) -> p a b', a=8)
        r_xo = ring[:, s_xo, :].rearrange('p (a b) -> p a b', a=8)
        rot = Rot(range(2, 16))
        for e in range(8):
            pa = rot.next()
            for c in range(8):
                mm(pst(pa), r_q[:, c, e * 128:(e + 1) * 128], hT2[:, c, :], c == 0, c == 7,
                   reads=[hkeys2[c], ('ring', s_q)], wkey=psk(pa), inc=(c == 7))
            S.op('act', lambda e=e, pa=pa: nc.scalar.mul(out=qT[:, e, :], in_=pst(pa), mul=1.0 / 16.0),
                 reads=[psk(pa)], writes=[('qT', e)])
            ts('dve', dd[:, e, :], qT[:, e, :], kmax[:, e:e + 1], None, ALU.mult, None, reads=[('qT', e), 'kmax'], writes=[('dd', e)])
            stt('dve', aq[:, e, :], dd[:, e, :], -1.0, dd[:, e, :], ALU.mult, ALU.max, reads=[('dd', e)], writes=[('aq', e)])
        for hd in range(4):
            pbd = rot.next()
            for ec in range(2):
                e = 2 * hd + ec
                mm(pst(pbd), onesb[:, :], aq[:, e, :], ec == 0, ec == 1, reads=[('aq', e), 'onesb'], wkey=psk(pbd), inc=(ec == 1))
            cp('act', bnd[:, hd, :], pst(pbd), reads=[psk(pbd)], writes=[('bnd', hd)])
        for hd in range(4):
            for mc in range(2):
                psc = rot.next()
                for ec in range(2):
                    e = 2 * hd + ec
                    mm(pst(psc), kT[:, e, mc * 128:(mc + 1) * 128], qT[:, e, :], ec == 0, ec == 1,
                       reads=[('kT', e), ('qT', e)], wkey=psk(psc), inc=(ec == 1))
                i2 = 2 * hd + mc
                tt('dve', dd[:, i2, :], pst(psc), bnd[:, hd, :], ALU.subtract, reads=[psk(psc), ('bnd', hd)], writes=[('dd', i2)])
                act(eT[:, i2, :], dd[:, i2, :], AF.Exp, reads=[('dd', i2)], writes=[('eT', i2)])
        for hd in range(4):
            psm = rot.next()
            for mc in range(2):
                i2 = 2 * hd + mc
                mm(pst(psm), onesb[:, :], eT[:, i2, :], mc == 0, mc == 1, reads=[('eT', i2), 'onesb'], wkey=psk(psm), inc=(mc == 1))
            S.op('dve', lambda psm=psm, hd=hd: nc.vector.reciprocal(out=rinv[:, hd, :], in_=pst(psm)), reads=[psk(psm)], writes=[('rinv', hd)])
        for hd in range(4):
            for ec in range(2):
                e = 2 * hd + ec
                po = rot.next()
                for mc in range(2):
                    i2 = 2 * hd + mc
                    mm(pst(po), vv[:, mc, e * 128:(e + 1) * 128], eT[:, i2, :], mc == 0, mc == 1,
                       reads=['vv', ('eT', i2)], wkey=psk(po), inc=(mc == 1))
                tt('dve', oT[:, e, :], pst(po), rinv[:, hd, :], ALU.mult, reads=[psk(po), ('rinv', hd)], writes=[('oT', e)])
        for e in range(8):
            pa = rot.next()
            for c in range(8):
                mm(pst(pa), r_xo[:, c, e * 128:(e + 1) * 128], oT[:, c, :], c == 0, c == 7,
                   reads=[('oT', c), ('ring', s_xo)], wkey=psk(pa), inc=(c == 7))
            tt('dve', xb[:, e, :], xb[:, e, :], pst(pa), ALU.add, reads=[xkeys[e], psk(pa)], writes=[xkeys[e]])

    peer_slots = {}

    def peer0(b):
        xb = xTs[b % 2]
        xkeys = xk(b)
        M.top = DYN
        S.phase(['qp', 'ss'])
        qp = M.alloc([128, 16, T], BF16)
        s_p0 = ring_load(U_PQ0)
        s_p1 = ring_load(U_PQ1)
        rmsnorm_to(xb, xkeys, PC_GPEER, hT, hkeys, T, 0)
        rot = Rot(range(2, 12))
        for cc in range(16):
            sl = s_p0 if cc < 8 else s_p1
            rp = ring[:, sl, :].rearrange('p (a b) -> p a b', a=8)
            pa = rot.next()
            for c in range(8):
                mm(pst(pa), rp[:, c, (cc % 8) * 128:(cc % 8 + 1) * 128], hT[:, c, :], c == 0, c == 7,
                   reads=[hkeys[c], ('ring', sl)], wkey=psk(pa), inc=(c == 7))
            cp('act', qp[:, cc, :], pst(pa), reads=[psk(pa)], writes=[('qp', cc)])
        for tt_ in range(2):
            tsl = slice(tt_ * 128, (tt_ + 1) * 128)
            for g4 in range(4):
                bank = 6 + g4 % 2
                for q4 in range(4):
                    cc = g4 * 4 + q4
                    mm(psb[bank][:, q4 * 128:(q4 + 1) * 128], qp[:, cc, tsl], skTb[:, cc % 2, :], True, True,
                       reads=[('qp', cc), 'skTb'], wkey=('psb', bank), inc=(q4 == 3))
                cp('act', ssb[:, tt_, g4 * 4:(g4 + 1) * 4, :], psb[bank][:, :].rearrange('p (a b) -> p a b', a=4),
                   reads=[('psb', bank)], writes=[('ss', tt_, g4)])

    def gen_topk(b, base):
        M.top = base
        tv = M.alloc([128, 8, 2, 16], F32)
        ti = M.alloc([128, 8, 2, 16], U32)
        cand = M.alloc([128, 8, 256], F32)
        code = M.alloc([128, 8, 256], U32)
        tish = M.alloc([128, 8, 16], U32)
        fv = M.alloc([128, 8, 16], F32)
        fa = M.alloc([128, 8, 16], U32)
        fb = M.alloc([128, 8, 16], U32)
        oh = cand.rearrange('p h (a b) -> p h a b', a=16)
        If = M.alloc([128, 128], F32)
        Jf = M.alloc([128, 128], F32)
        Gf = M.alloc([128, 128], F32)
        dsm = M.alloc([128, 8, 16], F32)
        esm = M.alloc([128, 8, 16], F32)
        zs = M.alloc([128, 8], F32)
        rz = M.alloc([128, 8], F32)
        tkend = M.top
        for tt_ in range(2):
            tsl = slice(tt_ * 128, (tt_ + 1) * 128)
            ss = ssb[:, tt_]
            ssu = ss.bitcast(U32)
            sall = [('ss', tt_, g) for g in range(4)]
            stt('dve', ssu, ssu, maskt[:, 8:9], iotau[:, :].unsqueeze(1).broadcast_to([128, 16, 128]), ALU.bitwise_and, ALU.bitwise_or,
                reads=sall + ['maskt', 'iotau'], writes=sall)
            yield
            for cc in range(16):
                h, p = cc // 2, cc % 2
                sk_ = ('ss', tt_, cc // 4)
                S.op('dve', lambda cc=cc, h=h, p=p: nc.vector.max(out=tv[:, h, p, 0:8], in_=ss[:, cc, :]),
                     reads=[sk_], writes=['tv'])
                S.op('dve', lambda cc=cc, h=h, p=p: nc.vector.match_replace(out=ss[:, cc, :], in_to_replace=tv[:, h, p, 0:8], in_values=ss[:, cc, :], imm_value=-1e30),
                     reads=[sk_, 'tv'], writes=[sk_], embed=False)
                S.op('dve', lambda cc=cc, h=h, p=p: nc.vector.max(out=tv[:, h, p, 8:16], in_=ss[:, cc, :]),
                     reads=[sk_], writes=['tv'])
                if cc % 2 == 1:
                    yield
            ts('dve', ti[:, :, :, :], tv.bitcast(U32), 127, None, ALU.bitwise_and, None, reads=['tv'], writes=['ti'])
            cand4 = cand.rearrange('p h (a b) -> p h a b', a=16)
            tt('dve', cand4, tv[:, :, 0, :].unsqueeze(3).broadcast_to([128, 8, 16, 16]),
               tv[:, :, 1, :].unsqueeze(2).broadcast_to([128, 8, 16, 16]), ALU.add, reads=['tv'], writes=['cand'])
            ts('dve', tish[:, :, :], ti[:, :, 0, :], 7, None, ALU.logical_shift_left, None, reads=['ti'], writes=['tish'])
            code4 = code.rearrange('p h (a b) -> p h a b', a=16)
            tt('dve', code4, tish[:, :, :].unsqueeze(3).broadcast_to([128, 8, 16, 16]),
               ti[:, :, 1, :].unsqueeze(2).broadcast_to([128, 8, 16, 16]), ALU.bitwise_or, reads=['tish', 'ti'], writes=['code'])
            candu = cand.bitcast(U32)
            stt('dve', candu, candu, maskt[:, 0:1], code[:, :, :], ALU.bitwise_and, ALU.bitwise_or,
                reads=['cand', 'maskt', 'code'], writes=['cand'])
            yield
            for h in range(8):
                S.op('dve', lambda h=h: nc.vector.max(out=fv[:, h, 0:8], in_=cand[:, h, :]), reads=['cand'], writes=['fv'])
                S.op('dve', lambda h=h: nc.vector.match_replace(out=cand[:, h, :], in_to_replace=fv[:, h, 0:8], in_values=cand[:, h, :], imm_value=-1e30),
                     reads=['cand', 'fv'], writes=['cand'], embed=False)
                S.op('dve', lambda h=h: nc.vector.max(out=fv[:, h, 8:16], in_=cand[:, h, :]), reads=['cand'], writes=['fv'])
                if h % 2 == 1:
                    yield
            tt('dve', dsm[:, :, :], fv[:, :, :], fv[:, :, 0:1].broadcast_to([128, 8, 16]), ALU.subtract, reads=['fv'], writes=['dsm'])
            act(esm[:, :, :], dsm[:, :, :], AF.Exp, reads=['dsm'], writes=['esm'])
            S.op('dve', lambda: nc.vector.reduce_sum(out=zs[:, :], in_=esm[:, :, :], axis=AX.X), reads=['esm'], writes=['zs'])
            S.op('dve', lambda: nc.vector.reciprocal(out=rz[:, :], in_=zs[:, :]), reads=['zs'], writes=['rz'])
            tt('dve', Gf.rearrange('p (h k) -> p h k', h=8), esm[:, :, :], rz[:, :].unsqueeze(2).broadcast_to([128, 8, 16]),
               ALU.mult, reads=['esm', 'rz'], writes=['Gf'])
            yield
            fvu = fv.bitcast(U32)
            ts('dve', fa[:, :, :], fvu, 7, 127, ALU.logical_shift_right, ALU.bitwise_and, reads=['fv'], writes=['fa'])
            ts('dve', fb[:, :, :], fvu, 127, None, ALU.bitwise_and, None, reads=['fv'], writes=['fb'])
            cp('dve', If.rearrange('p (h k) -> p h k', h=8), fa[:, :, :], reads=['fa'], writes=['If'])
            cp('dve', Jf.rearrange('p (h k) -> p h k', h=8), fb[:, :, :], reads=['fb'], writes=['Jf'])
            yield
            for (src, nm, dst) in ((If, 'If', Ism), (Jf, 'Jf', Jsm), (Gf, 'Gf', Gsm)):
                pa = 12 + (0 if nm == 'Jf' else 1)
                mm(pst(pa)[:, 0:128], src[:, :], ident, True, True, reads=[nm, 'par'], wkey=psk(pa), inc=True)
                cp('act', dst[:, tsl], pst(pa)[:, 0:128], reads=[psk(pa)], writes=[nm + 'sm'])
            yield

    def peer_wb_uv(b):
        xb = xTs[b % 2]
        xkeys = xk(b)
        M.top = SS_OFF
        S.phase(['GJ', 'AW', 'Wt', 'Aoh', 'Boh', 'ciota'])
        peer_slots.clear()
        peer_slots[0] = (ring_load(U_PEER + 0), ring_load(U_PEER + 1))
        peer_slots[1] = (ring_load(U_PEER + 2), ring_load(U_PEER + 3))
        GJ = M.alloc([128, 4, T], BF16)
        AW = M.alloc([128, 4, T], BF16)
        Wt = M.alloc([128, T, 128], BF16)
        TP = 8
        Aoh = M.alloc([128, 2, 128, TP], BF16)
        Boh = M.alloc([128, 2, 128, TP], BF16)
        ciota = M.alloc([128, 128, TP], BF16)
        cp('dve', ciota[:, :, :], iota.unsqueeze(2).broadcast_to([128, 128, TP]), reads=['par'], writes=['ciota'])
        for t0 in range(0, T, TP):
            ob = (t0 // TP) % 2

            def bc(v):
                return v[:, t0:t0 + TP].unsqueeze(1).broadcast_to([128, 128, TP])
            tt('dve', Aoh[:, ob, :, :], bc(Ism), ciota[:, :, :], ALU.is_equal, reads=['Ifsm', 'ciota'], writes=[('Aoh', ob)])
            tt('dve', Aoh[:, ob, :, :], Aoh[:, ob, :, :], bc(Gsm), ALU.mult, reads=[('Aoh', ob), 'Gfsm'], writes=[('Aoh', ob)])
            tt('dve', Boh[:, ob, :, :], bc(Jsm), ciota[:, :, :], ALU.is_equal, reads=['Jfsm', 'ciota'], writes=[('Boh', ob)])
            for t4 in range(0, TP, 4):
                bank = 6 + ((t0 + t4) // 4) % 2
                for q in range(4):
                    mm(psb[bank][:, q * 128:(q + 1) * 128], Aoh[:, ob, :, t4 + q], Boh[:, ob, :, t4 + q], True, True,
                       reads=[('Aoh', ob), ('Boh', ob)], wkey=('psb', bank), inc=(q == 3))
                cp('act', Wt[:, t0 + t4:t0 + t4 + 4, :], psb[bank][:, :].rearrange('p (a b) -> p a b', a=4),
                   reads=[('psb', bank)], writes=['Wt'])
        accs = list(range(0, 8))
        hrot = Rot([8, 10, 12, 14])
        NJ = 128
        PIPE = 2

        def u_side(j):
            ju, jj = j // 8, j % 8
            su = peer_slots[ju][0]
            ru = ring[:, su, :].rearrange('p (j c i) -> p j c i', j=8, c=8)
            ph = hrot.next()
            for c in range(8):
                mm(pst(ph), ru[:, jj, c, :], hT[:, c, :], c == 0, c == 7, reads=[hkeys[c], ('ring', su)], wkey=psk(ph), inc=(c == 7))
            b4 = j % 4
            act(GJ[:, b4, :], pst(ph), AF.Gelu, reads=[psk(ph)], writes=[('GJ', b4)])
            tt('dve', AW[:, b4, :], GJ[:, b4, :], Wt[:, :, j], ALU.mult, reads=[('GJ', b4), 'Wt'], writes=[('AW', b4)])

        def v_side(j):
            ju, jj = j // 8, j % 8
            sv = peer_slots[ju][1]
            rv = ring[:, sv, :].rearrange('p (j d) -> p j d', j=8)
            b4 = j % 4
            for e in range(8):
                S.op('pe', lambda e=e: nc.tensor.matmul(pst(accs[e]), lhsT=rv[:, jj, e * 128:(e + 1) * 128], rhs=AW[:, b4, :],
                                                        start=(j == 0 and e % 2 == 0), stop=(j == NJ - 1),
                                                        skip_group_check=True),
                     reads=[('AW', b4), ('ring', sv)], writes=[psk(accs[e])], inc=(e == 7))
        for j in range(NJ + PIPE):
            if j < NJ:
                u_side(j)
            if j >= PIPE:
                jv = j - PIPE
                v_side(jv)
                if jv % 8 == 7 and jv // 8 + 2 < 16:
                    g = jv // 8 + 2
                    peer_slots[g] = (ring_load(U_PEER + 2 * g), ring_load(U_PEER + 2 * g + 1))
        for e in range(8):
            tt('dve', xb[:, e, :], xb[:, e, :], pst(accs[e]), ALU.add, reads=[xkeys[e], psk(accs[e])], writes=[xkeys[e]])

    def final(b, base=None):
        xb = xTs[b % 2]
        xkeys = xk(b)
        if base is None:
            M.top = DYN
            S.phase(['yo'])
        else:
            M.top = base
        yo = M.alloc([128, 8, T], F32)
        okeys = [('yo', c) for c in range(8)]
        rmsnorm_to(xb, xkeys, PC_GFIN, yo, okeys, T, 0)
        S.dma('sp', out=outr[b], in_=yo.rearrange('p a b -> p (a b)'), reads=okeys, writes=[('out', b)])

    def drain(g):
        for _ in g:
            pass

    TK_BYTES = 25 * 1024
    load_x(0)
    S.phase(MIX_NAMES)
    drain(gen_mixer(0, DYN))
    if 'attn' in phases:
        attn(0)
    for b in range(nblk):
        if b + 1 < nblk:
            load_x(b + 1)
        if 'peer' in phases:
            peer0(b)
            S.phase(TK_NAMES + MIX_NAMES)
            g1 = gen_topk(b, DYN)
            g2 = gen_mixer(b + 1, DYN + TK_BYTES) if b + 1 < nblk else iter(())
            done1 = done2 = False
            while not (done1 and done2):
                if not done1:
                    try:
                        next(g1)
                    except StopIteration:
                        done1 = True
                if not done2:
                    try:
                        next(g2)
                    except StopIteration:
                        done2 = True
            peer_wb_uv(b)
        elif b + 1 < nblk:
            S.phase(MIX_NAMES)
            drain(gen_mixer(b + 1, DYN))
        if b + 1 < nblk and 'attn' in phases:
            attn(b + 1)
            final(b, base=DYN + 33 * 1024)
        else:
            final(b)
    S.barrier()
    es.close()
    return nc, S


def _prep_shared(inp):
    f = np.float32
    W = np.zeros((NUNITS, 128, UNIT), f)
    w_in = np.asarray(inp['w_in'][0], f)
    t = w_in.reshape(8, 128, 7, 8, 128)
    t = t.transpose(3, 1, 0, 2, 4).reshape(8, 128, 8 * 7 * 128)
    W[U_WIN:U_WIN + 8, :, :7168] = t

    def sq(w):
        w = np.asarray(w, f)
        return w.reshape(8, 128, w.shape[1]).transpose(1, 0, 2).reshape(128, -1)
    W[U_SCO] = sq(inp['sc_w_out'][0])
    W[U_PW2] = sq(inp['cf_w_pw2'][0])
    W[U_MIX] = sq(inp['w_mix_out'][0])
    W[U_WQ] = sq(inp['w_q'][0])
    W[U_WXO] = sq(inp['w_xo'][0])
    wpq = np.asarray(inp['w_peer_q'][0], f)
    W[U_PQ0] = sq(wpq[:, :1024])
    W[U_PQ1] = sq(wpq[:, 1024:])
    wkv = np.asarray(inp['w_kv'][0], f)
    W[U_WK] = sq(wkv[:, :1024])
    W[U_WV] = sq(wkv[:, 1024:])
    u = np.asarray(inp['peer_u'][0], f).reshape(128, 16, 8, 8, 128)
    u = u.transpose(1, 4, 2, 3, 0).reshape(16, 128, UNIT)
    v = np.asarray(inp['peer_v'][0], f).reshape(128, 16, 8, 1024)
    v = v.transpose(1, 0, 2, 3).reshape(16, 128, UNIT)
    W[U_PEER:U_PEER + 32:2] = u
    W[U_PEER + 1:U_PEER + 32:2] = v

    P = np.zeros((128, NPAR), f)

    def col(vv):
        return np.asarray(vv, f).reshape(8, 128).T
    P[:, PC_GMIX:PC_GMIX + 8] = col(inp['norm_mix_g'][0])
    b1 = np.asarray(inp['cf_b_pw1'][0], f)
    P[:, PC_B1A:PC_B1A + 8] = col(b1[:1024])
    P[:, PC_B1G:PC_B1G + 8] = col(b1[1024:])
    P[:, PC_CONVB:PC_CONVB + 8] = col(inp['cf_conv_b'][0])
    P[:, PC_LNG:PC_LNG + 8] = col(inp['cf_ln_g'][0])
    P[:, PC_LNB:PC_LNB + 8] = col(inp['cf_ln_b'][0])
    P[:, PC_BPW2:PC_BPW2 + 8] = col(inp['cf_b_pw2'][0])
    P[:, PC_GXA:PC_GXA + 8] = col(inp['norm_xa_g'][0])
    P[:, PC_GMEM:PC_GMEM + 8] = col(inp['norm_mem_g'][0])
    P[:, PC_GPEER:PC_GPEER + 8] = col(inp['norm_peer_g'][0])
    P[:, PC_GFIN:PC_GFIN + 8] = col(inp['final_norm_g'])
    scw = np.asarray(inp['sc_conv_w'][0], f)
    P[:, PC_SCW:PC_SCW + 24] = scw.reshape(3, 8, 128).transpose(2, 1, 0).reshape(128, 24)
    cfw = np.asarray(inp['cf_conv_w'][0], f)
    P[:, PC_CFW:PC_CFW + 248] = cfw.reshape(31, 8, 128).transpose(2, 1, 0).reshape(128, 248)
    sk = np.asarray(inp['peer_subkeys'][0], f)
    P[:, PC_SKT:PC_SKT + 256] = sk.transpose(2, 0, 1).reshape(128, 256)
    P[:, PC_IDENT:PC_IDENT + 128] = np.eye(128, dtype=f)
    P[:, PC_IOTA:PC_IOTA + 128] = np.arange(128, dtype=f)[None, :]
    return W, P


def _prep_core(x_b, mem_b):
    f = np.float32
    xb = np.asarray(x_b, f).reshape(NBLK, NB_TOK, 8, 128)
    xr = np.ascontiguousarray(xb.transpose(0, 3, 2, 1)).reshape(NBLK, 128, 8 * NB_TOK)
    mb = np.asarray(mem_b, f).reshape(NMEM, 8, 128)
    mr = np.ascontiguousarray(mb.transpose(2, 1, 0)).reshape(128, 8 * NMEM)
    return xr, mr


def _unprep_out(o):
    o = o.reshape(NBLK, 128, 8, NB_TOK)
    return np.ascontiguousarray(o.transpose(0, 3, 2, 1)).reshape(SEQ, D)


def kernel(**inputs):
    n = 8
    W, P = _prep_shared(inputs)
    nc, _ = build_nc()
    in_maps = []
    for b in range(n):
        xr, mr = _prep_core(inputs['x'][b], inputs['mem'][b])
        in_maps.append({"xr": xr, "memr": mr, "wunits": W, "params": P})
    res = run_bass_kernel_spmd(nc, in_maps, core_ids=list(range(n)))
    out = np.stack([_unprep_out(np.asarray(r["outr"])) for r in res.results], axis=0)
    return out.astype(np.float32)
```

```python
import contextlib
import numpy as np
import concourse.bass as bass
import concourse.mybir as mybir
from concourse.bass_utils import run_bass_kernel_spmd

F32 = mybir.dt.float32
BF16 = mybir.dt.bfloat16
U32 = mybir.dt.uint32
AF = mybir.ActivationFunctionType
ALU = mybir.AluOpType
AX = mybir.AxisListType

D = 1024
SEQ = 4096
NB_TOK = 256
NBLK = SEQ // NB_TOK
NMEM = 256
EPS = 1e-6
UNIT = 8192
U_WIN = 0
U_SCO, U_PW2, U_MIX, U_WQ, U_WXO, U_PQ0, U_PQ1, U_WK, U_WV = 8, 9, 10, 11, 12, 13, 14, 15, 16
U_PEER = 17
NUNITS = U_PEER + 32
PC_GMIX, PC_B1A, PC_B1G, PC_CONVB, PC_LNG, PC_LNB, PC_BPW2, PC_GXA, PC_GMEM, PC_GPEER, PC_GFIN = [8 * i for i in range(11)]
PC_SCW = 88
PC_CFW = PC_SCW + 24
PC_SKT = PC_CFW + 248
PC_IDENT = PC_SKT + 256
PC_IOTA = PC_IDENT + 128
NPAR = PC_IOTA + 128

SAME_ENG_SYNC = True


class Sched:
    ENG = ('pe', 'act', 'dve', 'pool', 'sp')

    def __init__(self, nc, es, nslots=24):
        self.nc = nc
        self.eng = {'pe': nc.tensor, 'act': nc.scalar, 'dve': nc.vector, 'pool': nc.gpsimd, 'sp': nc.sync}
        self.sem = {e: es.enter_context(nc.semaphore('sem_' + e)) for e in self.ENG}
        self.cnt = {e: 0 for e in self.ENG}
        self.dsem = [es.enter_context(nc.semaphore('dsem%d' % i)) for i in range(nslots)]
        self.dcnt = [0] * nslots
        self.dnext = 0
        self.seen = {e: {} for e in self.ENG}
        self.lastw = {}
        self.readers = {}
        self.nwaits = 0
        self.rkeys = set()
        self.prevR = {}
        self.curR = {}

    def phase(self, names):
        for k, v in self.curR.items():
            if self.prevR.get(k, 0) < v:
                self.prevR[k] = v
        self.curR = {}
        self.rkeys = set(names)

    def _touchR(self, reads, writes):
        for k in list(reads) + list(writes):
            nm = k if isinstance(k, str) else k[0]
            if nm in self.rkeys:
                return True
        return False

    def _semof(self, src):
        return self.dsem[src[1]] if isinstance(src, tuple) else self.sem[src]

    @staticmethod
    def _isps(k):
        return isinstance(k, tuple) and k[0] == 'psb'

    def _wait(self, e, deps, embed=False):
        need = {}
        for (src, val, isps) in deps:
            if src == e and (e == 'pe' or isps or not SAME_ENG_SYNC):
                continue
            if need.get(src, 0) < val:
                need[src] = val
        pend = [(src, val) for src, val in need.items() if self.seen[e].get(src, 0) < val]
        emb = pend.pop() if (embed and pend) else None
        for src, val in pend:
            self.eng[e].wait_ge(self._semof(src), val)
            self.nwaits += 1
        for src, val in pend + ([emb] if emb else []):
            self.seen[e][src] = val
        return emb

    def _deps(self, reads, writes):
        deps = []
        for k in reads:
            if self._isps(k):
                continue
            if k in self.lastw:
                deps.append(self.lastw[k] + (False,))
        for k in list(writes) + [k for k in reads if self._isps(k)]:
            ps = self._isps(k)
            if k in self.lastw:
                deps.append(self.lastw[k] + (ps,))
            r = self.readers.get(k)
            if r:
                deps.extend((a, b, ps) for a, b in r.items())
        return deps

    def _record(self, tag, reads, writes):
        writes = list(writes) + [k for k in reads if self._isps(k)]
        reads = [k for k in reads if not self._isps(k)]
        for k in writes:
            if self._isps(k) and k in self.lastw:
                r = self.readers.setdefault(k, {})
                lw = self.lastw[k]
                if lw[0] != tag[0] and r.get(lw[0], 0) < lw[1]:
                    r[lw[0]] = lw[1]
                r.pop(tag[0], None)
                self.lastw[k] = tag
            else:
                self.lastw[k] = tag
                self.readers[k] = {}
        for k in reads:
            r = self.readers.setdefault(k, {})
            if r.get(tag[0], 0) < tag[1]:
                r[tag[0]] = tag[1]

    def op(self, e, fn, reads=(), writes=(), inc=True, embed=True):
        deps = self._deps(reads, writes)
        tr = self._touchR(reads, writes)
        if tr:
            deps.extend((a, b, False) for a, b in self.prevR.items())
        emb = self._wait(e, deps, embed=(embed and e in ('act', 'dve')))
        ins = fn()
        if emb is not None:
            ins._wait_ge(self._semof(emb[0]), emb[1])
        if inc:
            self.cnt[e] += 1
            ins.then_inc(self.sem[e], 1)
            tag = (e, self.cnt[e])
        else:
            tag = (e, self.cnt[e] + 1)
        self._record(tag, reads, writes)
        if tr and self.curR.get(tag[0], 0) < tag[1]:
            self.curR[tag[0]] = tag[1]
        return ins

    def dma(self, q, out, in_, reads=(), writes=()):
        slot = self.dnext
        self.dnext = (self.dnext + 1) % len(self.dsem)
        deps = self._deps(reads, writes)
        tr = self._touchR(reads, writes)
        if tr:
            deps.extend((a, b, False) for a, b in self.prevR.items())
        if self.dcnt[slot] > 0:
            deps.append((('d', slot), self.dcnt[slot], False))
        self._wait(q, deps)
        ins = self.eng[q].dma_start(out=out, in_=in_)
        self.dcnt[slot] += 16
        ins.then_inc(self.dsem[slot], 16)
        tag = (('d', slot), self.dcnt[slot])
        self._record(tag, reads, writes)
        if tr:
            self.curR[tag[0]] = tag[1]
        return ins

    def barrier(self, engines=None):
        deps = [(e, self.cnt[e], False) for e in self.ENG if self.cnt[e] > 0]
        deps += [(('d', i), c, False) for i, c in enumerate(self.dcnt) if c > 0]
        for e in (engines or self.ENG):
            self._wait(e, [d for d in deps if d[0] != e])


class Mem:
    def __init__(self, big, nbytes):
        self.big = big
        self.n = nbytes
        self.top = 0

    def alloc(self, shape, dtype):
        esz = 2 if dtype == BF16 else 4
        n = int(np.prod(shape[1:])) * esz
        off = (self.top + 63) // 64 * 64
        assert off + n <= self.n, ("SBUF region overflow", off + n, self.n)
        self.top = off + n
        v = self.big[:, off // 4:(off + n) // 4]
        if dtype != F32:
            v = v.bitcast(dtype)
        if len(shape) > 2:
            names = 'abcd'[:len(shape) - 1]
            pat = 'p (%s) -> p %s' % (' '.join(names), ' '.join(names))
            v = v.rearrange(pat, **{nm: s for nm, s in zip(names, shape[1:])})
        return v


def build_nc(nblk=NBLK, phases=('mixer', 'attn', 'peer'), prepass=True, stop_at=None, nunits_pp=NUNITS):
    nc = bass.Bass("TRN2", target_bir_lowering=False)
    T = NB_TOK
    xr = nc.dram_tensor("xr", [NBLK, 128, 8 * T], F32, kind="ExternalInput").ap()
    memr = nc.dram_tensor("memr", [128, 8 * NMEM], F32, kind="ExternalInput").ap()
    wsrc = nc.dram_tensor("wunits", [NUNITS, 128, UNIT], F32, kind="ExternalInput").ap()
    parr = nc.dram_tensor("params", [128, NPAR], F32, kind="ExternalInput").ap()
    outr = nc.dram_tensor("outr", [NBLK, 128, 8 * T], F32, kind="ExternalOutput").ap()
    wscr = nc.dram_tensor("wscr", [NUNITS, 128, UNIT], BF16, kind="Internal").ap()

    es = contextlib.ExitStack()
    SB_BYTES = 212800
    big = es.enter_context(nc.sbuf_tensor("big", [128, SB_BYTES // 4], F32))
    psb = [es.enter_context(nc.psum_tensor("ps%d" % i, [128, 512], F32)) for i in range(8)]
    S = Sched(nc, es)
    M = Mem(big, SB_BYTES)

    par = M.alloc([128, NPAR], F32)
    xT = M.alloc([128, 8, T], F32)
    xT_b = M.alloc([128, 8, T], F32)
    hT = M.alloc([128, 8, T], BF16)
    hT2 = M.alloc([128, 8, T], BF16)
    kT = M.alloc([128, 8, NMEM], BF16)
    vv = M.alloc([128, 2, D], BF16)
    kmax = M.alloc([128, 8], F32)
    maskt = M.alloc([128, 16], U32)
    iotau = M.alloc([128, 128], U32)
    onesb = M.alloc([128, 128], BF16)
    skTb = M.alloc([128, 2, 128], BF16)
    prodbuf = M.alloc([128, 8, T + 2], BF16)
    glubuf = M.alloc([128, 8, T + 30], BF16)
    sqb = M.alloc([128, 8, T], BF16)
    rs0 = M.alloc([128, T], F32)
    rs1 = M.alloc([128, T], F32)
    ring = M.alloc([128, 4, UNIT], BF16)
    PTOP = M.top

    ident = par[:, PC_IDENT:PC_IDENT + 128]
    iota = par[:, PC_IOTA:PC_IOTA + 128]

    def pcol(base, k, n=1):
        return par[:, base + k:base + k + n]

    def pst(i):
        return psb[i // 2][:, (i % 2) * 256:(i % 2) * 256 + 256]

    def psk(i):
        return ('psb', i // 2)

    class Rot:
        def __init__(self, ids):
            self.ids = sorted(ids, key=lambda i: (i % 2, i // 2))
            self.i = 0

        def next(self):
            v = self.ids[self.i % len(self.ids)]
            self.i += 1
            return v

    ring_state = {'n': 0}

    def ring_load(unit):
        slot = ring_state['n'] % 4
        ring_state['n'] += 1
        S.dma('sp', out=ring[:, slot, :], in_=wscr[unit], reads=[('wscr', unit)], writes=[('ring', slot)])
        return slot

    def mm(out, lhsT, rhs, start, stop, reads, wkey, inc):
        return S.op('pe', lambda: nc.tensor.matmul(out, lhsT=lhsT, rhs=rhs, start=start, stop=stop),
                    reads=reads, writes=[wkey], inc=inc)

    def act(out, in_, func, reads, writes, bias=None, scale=None):
        kw = {}
        if bias is not None:
            kw['bias'] = bias
        if scale is not None:
            kw['scale'] = scale
        return S.op('act', lambda: nc.scalar.activation(out=out, in_=in_, func=func, **kw), reads=reads, writes=writes)

    def tt(e, out, in0, in1, op, reads, writes):
        return S.op(e, lambda: S.eng[e].tensor_tensor(out=out, in0=in0, in1=in1, op=op), reads=reads, writes=writes)

    def ts(e, out, in0, s1, s2, op0, op1, reads, writes):
        if op1 is None:
            return S.op(e, lambda: S.eng[e].tensor_scalar(out=out, in0=in0, scalar1=s1, scalar2=None, op0=op0),
                        reads=reads, writes=writes)
        return S.op(e, lambda: S.eng[e].tensor_scalar(out=out, in0=in0, scalar1=s1, scalar2=s2, op0=op0, op1=op1),
                    reads=reads, writes=writes)

    def stt(e, out, in0, scalar, in1, op0, op1, reads, writes):
        return S.op(e, lambda: S.eng[e].scalar_tensor_tensor(out=out, in0=in0, scalar=scalar, in1=in1, op0=op0, op1=op1),
                    reads=reads, writes=writes)

    def cp(e, out, in_, reads, writes):
        if e == 'act':
            return S.op('act', lambda: nc.scalar.copy(out=out, in_=in_), reads=reads, writes=writes)
        return S.op(e, lambda: S.eng[e].tensor_copy(out=out, in_=in_), reads=reads, writes=writes)

    def rmsnorm_to(src, srckeys, gbase, dst, dstkeys, ncols, pt, out_dtype_is_bf16=True):
        for c in range(8):
            act(sqb[:, c, 0:ncols], src[:, c, :], AF.Square, reads=[srckeys[c]], writes=[('sqb', c)])
        for c in range(8):
            mm(pst(pt)[:, 0:ncols], onesb[:, :], sqb[:, c, 0:ncols], c == 0, c == 7,
               reads=[('sqb', c), 'onesb'], wkey=psk(pt), inc=(c == 7))
        act(rs0[:, 0:ncols], pst(pt)[:, 0:ncols], AF.Sqrt, reads=[psk(pt)], writes=['rs0'], bias=EPS, scale=1.0 / D)
        S.op('dve', lambda: nc.vector.reciprocal(out=rs1[:, 0:ncols], in_=rs0[:, 0:ncols]), reads=['rs0'], writes=['rs1'])
        for c in range(8):
            stt('dve', dst[:, c, :], src[:, c, :], pcol(gbase, c), rs1[:, 0:ncols], ALU.mult, ALU.mult,
                reads=[srckeys[c], 'rs1', 'par'], writes=[dstkeys[c]])

    S.dma('sp', out=par[:, :], in_=parr[:, :], writes=['par'])
    S.op('pool', lambda: nc.gpsimd.memset(onesb[:, :], 1.0), writes=['onesb'])
    S.op('dve', lambda: nc.vector.memset(maskt[:, 0:8], 0xFFFFC000), writes=['maskt'])
    S.op('dve', lambda: nc.vector.memset(maskt[:, 8:16], 0xFFFFFF80), writes=['maskt'])
    cp('dve', iotau[:, :], iota, reads=['par', 'maskt'], writes=['iotau'])
    cp('dve', skTb[:, :, :], par[:, PC_SKT:PC_SKT + 256].rearrange('p (a b) -> p a b', a=2), reads=['par'], writes=['skTb'])
    S.op('pool', lambda: nc.gpsimd.memset(prodbuf[:, :, :], 0.0), writes=['prodbuf'])
    S.op('pool', lambda: nc.gpsimd.memset(glubuf[:, :, :], 0.0), writes=['glubuf'])
    if stop_at == 'consts':
        S.barrier(); es.close(); return nc, S
    if prepass:
        M.top = PTOP
        S.phase(['stage'])
        NST = 2
        stage = M.alloc([128, NST, UNIT], F32)
        cuts = [0, 2944, 5888, UNIT]
        cengs = ['act', 'dve', 'pool']
        order = [U_WK, U_WV] + [u for u in range(nunits_pp) if u not in (U_WK, U_WV)]
        order = [u for u in order if u < nunits_pp or u in (U_WK, U_WV)]

        def pp_load(i):
            S.dma('sp', out=stage[:, i % NST, :], in_=wsrc[order[i]], writes=[('stage', i % NST)])
        for i in range(min(NST, len(order))):
            pp_load(i)
        for i, u in enumerate(order):
            sb = i % NST
            slot = i % 4
            for pi in range(3):
                a, b = cuts[pi], cuts[pi + 1]
                cp(cengs[pi], ring[:, slot, a:b], stage[:, sb, a:b], reads=[('stage', sb)], writes=[('ring', slot)])
            S.dma('sp', out=wscr[u], in_=ring[:, slot, :], reads=[('ring', slot)], writes=[('wscr', u)])
            if i + NST < len(order):
                pp_load(i + NST)
        ring_state['n'] = len(order) % 4
    if stop_at == 'prepass':
        S.barrier(); es.close(); return nc, S
    M.top = PTOP
    S.phase(['mT', 'absk'])
    mT = M.alloc([128, 8, NMEM], F32)
    absk = M.alloc([128, NMEM], F32)
    S.dma('sp', out=mT.rearrange('p a b -> p (a b)'), in_=memr[:, :], writes=[('mT', c) for c in range(8)])
    sK = ring_load(U_WK)
    sV = ring_load(U_WV)
    rmsnorm_to(mT, [('mT', c) for c in range(8)], PC_GMEM, hT, [('hT', c) for c in range(8)], NMEM, 0)
    if stop_at == 'kv1':
        S.barrier(); es.close(); return nc, S
    rK = ring[:, sK, :].rearrange('p (a b) -> p a b', a=8)
    rV = ring[:, sV, :].rearrange('p (a b) -> p a b', a=8)
    hkeys = [('hT', c) for c in range(8)]
    rot = Rot(range(2, 12))
    for e in range(8):
        pt = rot.next()
        for dc in range(8):
            mm(pst(pt), rK[:, dc, e * 128:(e + 1) * 128], hT[:, dc, :], dc == 0, dc == 7,
               reads=[hkeys[dc], ('ring', sK)], wkey=psk(pt), inc=(dc == 7))
        import os
        SK = os.environ.get('KV_SKIP', '')
        if 'act' not in SK:
            cp('act', kT[:, e, :], pst(pt), reads=[psk(pt)], writes=[('kT', e)])
        if 'dve' not in SK:
            stt('dve', absk[:, :], kT[:, e, :], -1.0, kT[:, e, :], ALU.mult, ALU.max, reads=[('kT', e)], writes=['absk'])
        if 'red' not in SK:
            S.op('dve', lambda e=e: nc.vector.reduce_max(out=kmax[:, e:e + 1], in_=absk[:, :], axis=AX.X),
                 reads=['absk'], writes=['kmax'])
    if stop_at == 'kv2':
        S.barrier(); es.close(); return nc, S
    for mc in range(2):
        for eh in range(2):
            bank = 6 + (mc * 2 + eh) % 2
            for dc in range(8):
                mm(psb[bank][:, :], hT[:, dc, mc * 128:(mc + 1) * 128], rV[:, dc, eh * 512:(eh + 1) * 512], dc == 0, dc == 7,
                   reads=[hkeys[dc], ('ring', sV)], wkey=('psb', bank), inc=(dc == 7))
            cp('act', vv[:, mc, eh * 512:(eh + 1) * 512], psb[bank][:, :], reads=[('psb', bank)], writes=['vv'])

    if stop_at == 'kv':
        S.barrier(); es.close(); return nc, S

    T_ = T
    xTs = [xT, xT_b]
    M.top = PTOP
    LISTS = M.alloc([128, 3, T], BF16)
    Ism, Jsm, Gsm = LISTS[:, 0, :], LISTS[:, 1, :], LISTS[:, 2, :]
    SS_OFF = M.top
    ssb = M.alloc([128, 2, 16, 128], F32)
    DYN = M.top

    def xk(b):
        return [('xT', b % 2, c) for c in range(8)]
    hkeys2 = [('hT2', c) for c in range(8)]

    def load_x(b):
        S.dma('sp', out=xTs[b % 2].rearrange('p a b -> p (a b)'), in_=xr[b], writes=xk(b))

    MIX_NAMES = ['ysc', 'zz', 'gsc', 'gcf', 'cfa', 'mg', 'scc', 'scb', 'sg', 'ycf', 'm1', 'm2', 'zb', 'z2b', 'dcf', 'dsc1',
                 'mean', 'msq', 'var', 'lrs']
    TK_NAMES = ['ss', 'tv', 'ti', 'tif', 'cand', 'code', 'tish', 'fv', 'fpos', 'fa', 'fb', 'faf', 'fbf', 'oh', 'If', 'Jf', 'Gf', 'dsm', 'esm',
                'zs', 'rz']

    def gen_mixer(b, base):
        xb = xTs[b % 2]
        xkeys = xk(b)
        M.top = base
        ysc = M.alloc([128, 8, T], BF16)
        zz = M.alloc([128, 8, T], F32)
        gsc = M.alloc([128, 8, T], BF16)
        gcf = M.alloc([128, 8, T], BF16)
        cfa = M.alloc([128, 8, T], BF16)
        merged = M.alloc([128, 8, T], BF16)
        scc = M.alloc([128, T], F32)
        scb = M.alloc([128, T], F32)
        sg = M.alloc([128, T], F32)
        ycf = M.alloc([128, T], F32)
        m1 = M.alloc([128, T], F32)
        zb = M.alloc([128, 2, T], BF16)
        z2b = M.alloc([128, 2, T], BF16)
        dcf = M.alloc([128, 1, 31, 128], BF16)
        dsc1 = M.alloc([128, 2, 3, 128], BF16)
        mean = M.alloc([128, T], F32)
        msq = M.alloc([128, T], F32)
        var = M.alloc([128, T], F32)
        lrs = M.alloc([128, T], F32)
        if b > 0:
            cp('pool', prodbuf[:, :, 0:2], prodbuf[:, :, T:T + 2], reads=['prodbuf'], writes=['prodbuf'])
            cp('pool', glubuf[:, :, 0:30], glubuf[:, :, T:T + 30], reads=['glubuf'], writes=['glubuf'])
        rmsnorm_to(xb, xkeys, PC_GMIX, hT2, hkeys2, T, 0)
        yield
        rot = Rot(range(2, 12))
        PS_SUM, PS_SQ = 14, 1
        slots = {}
        slots[0] = ring_load(U_WIN + 0)
        slots[1] = ring_load(U_WIN + 1)

        def stats(kk):
            d2 = kk % 2
            mm(pst(PS_SUM), onesb[:, :], zb[:, d2, :], kk == 0, kk == 7, reads=[('zb', d2), 'onesb'], wkey=psk(PS_SUM), inc=True)
            mm(pst(PS_SQ), onesb[:, :], z2b[:, d2, :], kk == 0, kk == 7, reads=[('z2b', d2), 'onesb'], wkey=psk(PS_SQ), inc=True)
        for k in range(8):
            if k + 2 < 8:
                slots[k + 2] = ring_load(U_WIN + k + 2)
            sl = slots[k]
            rw = ring[:, sl, 0:7168].rearrange('p (a f c) -> p a f c', a=8, f=7)
            db = k % 2
            tt('pool', dcf[:, 0, :, :], ident.unsqueeze(1).broadcast_to([128, 31, 128]),
               par[:, PC_CFW + 31 * k:PC_CFW + 31 * k + 31].unsqueeze(2).broadcast_to([128, 31, 128]), ALU.mult,
               reads=['par'], writes=[('dcf', 0)])
            tt('pool', dsc1[:, db, :, :], ident.unsqueeze(1).broadcast_to([128, 3, 128]),
               par[:, PC_SCW + 3 * k:PC_SCW + 3 * k + 3].unsqueeze(2).broadcast_to([128, 3, 128]), ALU.mult,
               reads=['par'], writes=[('dsc1', db)])

            def proj(fam):
                pt = rot.next()
                for dc in range(8):
                    mm(pst(pt), rw[:, dc, fam, :], hT2[:, dc, :], dc == 0, dc == 7,
                       reads=[hkeys2[dc], ('ring', sl)], wkey=psk(pt), inc=(dc == 7))
                return pt
            p_c = proj(1)
            cp('act', scc[:, :], pst(p_c), reads=[psk(p_c)], writes=['scc'])
            p_x = proj(2)
            tt('dve', prodbuf[:, k, 2:T + 2], pst(p_x), scc[:, :], ALU.mult, reads=[psk(p_x), 'scc'], writes=['prodbuf'])
            yield
            p_g = proj(4)
            act(sg[:, :], pst(p_g), AF.Sigmoid, reads=[psk(p_g), 'par'], writes=['sg'], bias=pcol(PC_B1G, k))
            p_a = proj(3)
            stt('dve', glubuf[:, k, 30:T + 30], pst(p_a), pcol(PC_B1A, k), sg[:, :], ALU.add, ALU.mult,
                reads=[psk(p_a), 'sg', 'par'], writes=['glubuf'])
            yield
            p_b = proj(0)
            cp('act', scb[:, :], pst(p_b), reads=[psk(p_b)], writes=['scb'])
            p_cv = rot.next()
            for tap in range(3):
                mm(pst(p_cv), dsc1[:, db, tap, :], prodbuf[:, k, tap:tap + T], tap == 0, tap == 2,
                   reads=['prodbuf', ('dsc1', db)], wkey=psk(p_cv), inc=(tap == 2))
            tt('dve', ysc[:, k, :], pst(p_cv), scb[:, :], ALU.mult, reads=[psk(p_cv), 'scb'], writes=[('ysc', k)])
            yield
            p_1 = proj(5)
            act(gsc[:, k, :], pst(p_1), AF.Sigmoid, reads=[psk(p_1)], writes=[('gsc', k)])
            p_2 = proj(6)
            act(gcf[:, k, :], pst(p_2), AF.Sigmoid, reads=[psk(p_2)], writes=[('gcf', k)])
            yield
            p_cv = rot.next()
            for tap in range(31):
                mm(pst(p_cv), dcf[:, 0, tap, :], glubuf[:, k, tap:tap + T], tap == 0, tap == 30,
                   reads=['glubuf', ('dcf', 0)], wkey=psk(p_cv), inc=(tap == 30))
            act(zz[:, k, :], pst(p_cv), AF.Identity, reads=[psk(p_cv), 'par'], writes=[('zz', k)], bias=pcol(PC_CONVB, k))
            act(z2b[:, db, :], pst(p_cv), AF.Square, reads=[psk(p_cv), 'par'], writes=[('z2b', db)], bias=pcol(PC_CONVB, k))
            cp('dve', zb[:, db, :], zz[:, k, :], reads=[('zz', k)], writes=[('zb', db)])
            if k > 0:
                stats(k - 1)
            yield
        stats(7)
        s_sco = ring_load(U_SCO)
        s_pw2 = ring_load(U_PW2)
        ts('dve', mean[:, :], pst(PS_SUM), 1.0 / D, None, ALU.mult, None, reads=[psk(PS_SUM)], writes=['mean'])
        tt('dve', msq[:, :], mean[:, :], mean[:, :], ALU.mult, reads=['mean'], writes=['msq'])
        stt('dve', var[:, :], pst(PS_SQ), 1.0 / D, msq[:, :], ALU.mult, ALU.subtract, reads=[psk(PS_SQ), 'msq'], writes=['var'])
        act(msq[:, :], var[:, :], AF.Sqrt, reads=['var'], writes=['msq'], bias=EPS, scale=1.0)
        S.op('dve', lambda: nc.vector.reciprocal(out=lrs[:, :], in_=msq[:, :]), reads=['msq'], writes=['lrs'])
        yield
        for k in range(8):
            tt('dve', zz[:, k, :], zz[:, k, :], mean[:, :], ALU.subtract, reads=[('zz', k), 'mean'], writes=[('zz', k)])
            tt('dve', zz[:, k, :], zz[:, k, :], lrs[:, :], ALU.mult, reads=[('zz', k), 'lrs'], writes=[('zz', k)])
            act(cfa[:, k, :], zz[:, k, :], AF.Silu, reads=[('zz', k), 'par'], writes=[('cfa', k)],
                bias=pcol(PC_LNB, k), scale=pcol(PC_LNG, k))
            if k % 2 == 1:
                yield
        r_sco = ring[:, s_sco, :].rearrange('p (a b) -> p a b', a=8)
        r_pw2 = ring[:, s_pw2, :].rearrange('p (a b) -> p a b', a=8)
        s_mix = ring_load(U_MIX)
        for e in range(8):
            pa = rot.next()
            for c in range(8):
                mm(pst(pa), r_sco[:, c, e * 128:(e + 1) * 128], ysc[:, c, :], c == 0, c == 7,
                   reads=[('ysc', c), ('ring', s_sco)], wkey=psk(pa), inc=(c == 7))
            pb = rot.next()
            for c in range(8):
                mm(pst(pb), r_pw2[:, c, e * 128:(e + 1) * 128], cfa[:, c, :], c == 0, c == 7,
                   reads=[('cfa', c), ('ring', s_pw2)], wkey=psk(pb), inc=(c == 7))
            act(ycf[:, :], pst(pb), AF.Identity, reads=[psk(pb), 'par'], writes=['ycf'], bias=pcol(PC_BPW2, e))
            tt('dve', m1[:, :], pst(pa), gsc[:, e, :], ALU.mult, reads=[psk(pa), ('gsc', e)], writes=['m1'])
            tt('dve', ycf[:, :], ycf[:, :], gcf[:, e, :], ALU.mult, reads=['ycf', ('gcf', e)], writes=['ycf'])
            tt('dve', merged[:, e, :], m1[:, :], ycf[:, :], ALU.add, reads=['m1', 'ycf'], writes=[('mg', e)])
            yield
        r_mix = ring[:, s_mix, :].rearrange('p (a b) -> p a b', a=8)
        for e in range(8):
            pa = rot.next()
            for c in range(8):
                mm(pst(pa), r_mix[:, c, e * 128:(e + 1) * 128], merged[:, c, :], c == 0, c == 7,
                   reads=[('mg', c), ('ring', s_mix)], wkey=psk(pa), inc=(c == 7))
            tt('dve', xb[:, e, :], xb[:, e, :], pst(pa), ALU.add, reads=[xkeys[e], psk(pa)], writes=[xkeys[e]])
            if e % 2 == 1:
                yield

    def attn(b):
        xb = xTs[b % 2]
        xkeys = xk(b)
        M.top = DYN
        S.phase(['qT', 'oT', 'aq', 'bnd', 'dd', 'eT', 'rinv'])
        qT = M.alloc([128, 8, T], BF16)
        oT = M.alloc([128, 8, T], BF16)
        aq = M.alloc([128, 8, T], BF16)
        bnd = M.alloc([128, 4, T], F32)
        dd = M.alloc([128, 8, T], F32)
        eT = M.alloc([128, 8, T], BF16)
        rinv = M.alloc([128, 4, T], F32)
        if ('att', b) in pre:
            s_q, s_xo = pre.pop(('att', b))
        else:
            s_q = ring_load(U_WQ)
            s_xo = ring_load(U_WXO)
        rmsnorm_to(xb, xkeys, PC_GXA, hT2, hkeys2, T, 0)
        r_q = ring[:, s_q, :].rearrange('p (a b) -> p a b', a=8)
        r_xo = ring[:, s_xo, :].rearrange('p (a b) -> p a b', a=8)
        rot = Rot(range(2, 16))
        for e in range(8):
            pa = rot.next()
            for c in range(8):
                mm(pst(pa), r_q[:, c, e * 128:(e + 1) * 128], hT2[:, c, :], c == 0, c == 7,
                   reads=[hkeys2[c], ('ring', s_q)], wkey=psk(pa), inc=(c == 7))
            S.op('act', lambda e=e, pa=pa: nc.scalar.mul(out=qT[:, e, :], in_=pst(pa), mul=1.0 / 16.0),
                 reads=[psk(pa)], writes=[('qT', e)])
            ts('dve', dd[:, e, :], qT[:, e, :], kmax[:, e:e + 1], None, ALU.mult, None, reads=[('qT', e), 'kmax'], writes=[('dd', e)])
            stt('dve', aq[:, e, :], dd[:, e, :], -1.0, dd[:, e, :], ALU.mult, ALU.max, reads=[('dd', e)], writes=[('aq', e)])
        for hd in range(4):
            pbd = rot.next()
            for ec in range(2):
                e = 2 * hd + ec
                mm(pst(pbd), onesb[:, :], aq[:, e, :], ec == 0, ec == 1, reads=[('aq', e), 'onesb'], wkey=psk(pbd), inc=(ec == 1))
            cp('act', bnd[:, hd, :], pst(pbd), reads=[psk(pbd)], writes=[('bnd', hd)])
        for hd in range(4):
            for mc in range(2):
                psc = rot.next()
                for ec in range(2):
                    e = 2 * hd + ec
                    mm(pst(psc), kT[:, e, mc * 128:(mc + 1) * 128], qT[:, e, :], ec == 0, ec == 1,
                       reads=[('kT', e), ('qT', e)], wkey=psk(psc), inc=(ec == 1))
                i2 = 2 * hd + mc
                tt('dve', dd[:, i2, :], pst(psc), bnd[:, hd, :], ALU.subtract, reads=[psk(psc), ('bnd', hd)], writes=[('dd', i2)])
                act(eT[:, i2, :], dd[:, i2, :], AF.Exp, reads=[('dd', i2)], writes=[('eT', i2)])
        for hd in range(4):
            psm = rot.next()
            for mc in range(2):
                i2 = 2 * hd + mc
                mm(pst(psm), onesb[:, :], eT[:, i2, :], mc == 0, mc == 1, reads=[('eT', i2), 'onesb'], wkey=psk(psm), inc=(mc == 1))
            S.op('dve', lambda psm=psm, hd=hd: nc.vector.reciprocal(out=rinv[:, hd, :], in_=pst(psm)), reads=[psk(psm)], writes=[('rinv', hd)])
        for hd in range(4):
            for ec in range(2):
                e = 2 * hd + ec
                po = rot.next()
                for mc in range(2):
                    i2 = 2 * hd + mc
                    mm(pst(po), vv[:, mc, e * 128:(e + 1) * 128], eT[:, i2, :], mc == 0, mc == 1,
                       reads=['vv', ('eT', i2)], wkey=psk(po), inc=(mc == 1))
                tt('dve', oT[:, e, :], pst(po), rinv[:, hd, :], ALU.mult, reads=[psk(po), ('rinv', hd)], writes=[('oT', e)])
        for e in range(8):
            pa = rot.next()
            for c in range(8):
                mm(pst(pa), r_xo[:, c, e * 128:(e + 1) * 128], oT[:, c, :], c == 0, c == 7,
                   reads=[('oT', c), ('ring', s_xo)], wkey=psk(pa), inc=(c == 7))
            tt('dve', xb[:, e, :], xb[:, e, :], pst(pa), ALU.add, reads=[xkeys[e], psk(pa)], writes=[xkeys[e]])

    peer_slots = {}
    pre = {}

    def peer0(b):
        xb = xTs[b % 2]
        xkeys = xk(b)
        M.top = DYN
        S.phase(['qp', 'ss'])
        qp = M.alloc([128, 16, T], BF16)
        if ('pq', b) in pre:
            s_p0, s_p1 = pre.pop(('pq', b))
        else:
            s_p0 = ring_load(U_PQ0)
            s_p1 = ring_load(U_PQ1)
        rmsnorm_to(xb, xkeys, PC_GPEER, hT, hkeys, T, 0)
        rot = Rot(range(2, 12))
        for cc in range(16):
            sl = s_p0 if cc < 8 else s_p1
            rp = ring[:, sl, :].rearrange('p (a b) -> p a b', a=8)
            pa = rot.next()
            for c in range(8):
                mm(pst(pa), rp[:, c, (cc % 8) * 128:(cc % 8 + 1) * 128], hT[:, c, :], c == 0, c == 7,
                   reads=[hkeys[c], ('ring', sl)], wkey=psk(pa), inc=(c == 7))
            cp('act', qp[:, cc, :], pst(pa), reads=[psk(pa)], writes=[('qp', cc)])
        for tt_ in range(2):
            tsl = slice(tt_ * 128, (tt_ + 1) * 128)
            for g4 in range(4):
                bank = 6 + g4 % 2
                for q4 in range(4):
                    cc = g4 * 4 + q4
                    mm(psb[bank][:, q4 * 128:(q4 + 1) * 128], qp[:, cc, tsl], skTb[:, cc % 2, :], True, True,
                       reads=[('qp', cc), 'skTb'], wkey=('psb', bank), inc=(q4 == 3))
                cp('act', ssb[:, tt_, g4 * 4:(g4 + 1) * 4, :], psb[bank][:, :].rearrange('p (a b) -> p a b', a=4),
                   reads=[('psb', bank)], writes=[('ss', tt_, g4)])

    def gen_topk(b, base):
        M.top = base
        tv = M.alloc([128, 8, 2, 16], F32)
        ti = M.alloc([128, 8, 2, 16], U32)
        cand = M.alloc([128, 8, 256], F32)
        code = M.alloc([128, 8, 256], U32)
        tish = M.alloc([128, 8, 16], U32)
        fv = M.alloc([128, 8, 16], F32)
        fa = M.alloc([128, 8, 16], U32)
        fb = M.alloc([128, 8, 16], U32)
        oh = cand.rearrange('p h (a b) -> p h a b', a=16)
        If = M.alloc([128, 128], F32)
        Jf = M.alloc([128, 128], F32)
        Gf = M.alloc([128, 128], F32)
        dsm = M.alloc([128, 8, 16], F32)
        esm = M.alloc([128, 8, 16], F32)
        zs = M.alloc([128, 8], F32)
        rz = M.alloc([128, 8], F32)
        tkend = M.top
        for tt_ in range(2):
            tsl = slice(tt_ * 128, (tt_ + 1) * 128)
            ss = ssb[:, tt_]
            ssu = ss.bitcast(U32)
            sall = [('ss', tt_, g) for g in range(4)]
            stt('dve', ssu, ssu, maskt[:, 8:9], iotau[:, :].unsqueeze(1).broadcast_to([128, 16, 128]), ALU.bitwise_and, ALU.bitwise_or,
                reads=sall + ['maskt', 'iotau'], writes=sall)
            yield
            for cc in range(16):
                h, p = cc // 2, cc % 2
                sk_ = ('ss', tt_, cc // 4)
                S.op('dve', lambda cc=cc, h=h, p=p: nc.vector.max(out=tv[:, h, p, 0:8], in_=ss[:, cc, :]),
                     reads=[sk_], writes=['tv'])
                S.op('dve', lambda cc=cc, h=h, p=p: nc.vector.match_replace(out=ss[:, cc, :], in_to_replace=tv[:, h, p, 0:8], in_values=ss[:, cc, :], imm_value=-1e30),
                     reads=[sk_, 'tv'], writes=[sk_], embed=False)
                S.op('dve', lambda cc=cc, h=h, p=p: nc.vector.max(out=tv[:, h, p, 8:16], in_=ss[:, cc, :]),
                     reads=[sk_], writes=['tv'])
                if cc % 2 == 1:
                    yield
            ts('dve', ti[:, :, :, :], tv.bitcast(U32), 127, None, ALU.bitwise_and, None, reads=['tv'], writes=['ti'])
            cand4 = cand.rearrange('p h (a b) -> p h a b', a=16)
            tt('dve', cand4, tv[:, :, 0, :].unsqueeze(3).broadcast_to([128, 8, 16, 16]),
               tv[:, :, 1, :].unsqueeze(2).broadcast_to([128, 8, 16, 16]), ALU.add, reads=['tv'], writes=['cand'])
            ts('dve', tish[:, :, :], ti[:, :, 0, :], 7, None, ALU.logical_shift_left, None, reads=['ti'], writes=['tish'])
            code4 = code.rearrange('p h (a b) -> p h a b', a=16)
            tt('dve', code4, tish[:, :, :].unsqueeze(3).broadcast_to([128, 8, 16, 16]),
               ti[:, :, 1, :].unsqueeze(2).broadcast_to([128, 8, 16, 16]), ALU.bitwise_or, reads=['tish', 'ti'], writes=['code'])
            candu = cand.bitcast(U32)
            stt('dve', candu, candu, maskt[:, 0:1], code[:, :, :], ALU.bitwise_and, ALU.bitwise_or,
                reads=['cand', 'maskt', 'code'], writes=['cand'])
            yield
            for h in range(8):
                S.op('dve', lambda h=h: nc.vector.max(out=fv[:, h, 0:8], in_=cand[:, h, :]), reads=['cand'], writes=['fv'])
                S.op('dve', lambda h=h: nc.vector.match_replace(out=cand[:, h, :], in_to_replace=fv[:, h, 0:8], in_values=cand[:, h, :], imm_value=-1e30),
                     reads=['cand', 'fv'], writes=['cand'], embed=False)
                S.op('dve', lambda h=h: nc.vector.max(out=fv[:, h, 8:16], in_=cand[:, h, :]), reads=['cand'], writes=['fv'])
                if h % 2 == 1:
                    yield
            tt('dve', dsm[:, :, :], fv[:, :, :], fv[:, :, 0:1].broadcast_to([128, 8, 16]), ALU.subtract, reads=['fv'], writes=['dsm'])
            act(esm[:, :, :], dsm[:, :, :], AF.Exp, reads=['dsm'], writes=['esm'])
            S.op('dve', lambda: nc.vector.reduce_sum(out=zs[:, :], in_=esm[:, :, :], axis=AX.X), reads=['esm'], writes=['zs'])
            S.op('dve', lambda: nc.vector.reciprocal(out=rz[:, :], in_=zs[:, :]), reads=['zs'], writes=['rz'])
            tt('dve', Gf.rearrange('p (h k) -> p h k', h=8), esm[:, :, :], rz[:, :].unsqueeze(2).broadcast_to([128, 8, 16]),
               ALU.mult, reads=['esm', 'rz'], writes=['Gf'])
            yield
            fvu = fv.bitcast(U32)
            ts('dve', fa[:, :, :], fvu, 7, 127, ALU.logical_shift_right, ALU.bitwise_and, reads=['fv'], writes=['fa'])
            ts('dve', fb[:, :, :], fvu, 127, None, ALU.bitwise_and, None, reads=['fv'], writes=['fb'])
            cp('dve', If.rearrange('p (h k) -> p h k', h=8), fa[:, :, :], reads=['fa'], writes=['If'])
            cp('dve', Jf.rearrange('p (h k) -> p h k', h=8), fb[:, :, :], reads=['fb'], writes=['Jf'])
            yield
            for (src, nm, dst) in ((If, 'If', Ism), (Jf, 'Jf', Jsm), (Gf, 'Gf', Gsm)):
                pa = 12 + (0 if nm == 'Jf' else 1)
                mm(pst(pa)[:, 0:128], src[:, :], ident, True, True, reads=[nm, 'par'], wkey=psk(pa), inc=True)
                cp('act', dst[:, tsl], pst(pa)[:, 0:128], reads=[psk(pa)], writes=[nm + 'sm'])
            yield

    def peer_wb_uv(b):
        xb = xTs[b % 2]
        xkeys = xk(b)
        M.top = SS_OFF
        S.phase(['GJ', 'AW', 'Wt', 'Aoh', 'Boh', 'ciota'])
        peer_slots.clear()
        peer_slots[0] = (ring_load(U_PEER + 0), ring_load(U_PEER + 1))
        peer_slots[1] = (ring_load(U_PEER + 2), ring_load(U_PEER + 3))
        GJ = M.alloc([128, 4, T], BF16)
        AW = M.alloc([128, 4, T], BF16)
        Wt = M.alloc([128, T, 128], BF16)
        TP = 8
        Aoh = M.alloc([128, 2, 128, TP], BF16)
        Boh = M.alloc([128, 2, 128, TP], BF16)
        ciota = M.alloc([128, 128, TP], BF16)
        cp('dve', ciota[:, :, :], iota.unsqueeze(2).broadcast_to([128, 128, TP]), reads=['par'], writes=['ciota'])
        for t0 in range(0, T, TP):
            ob = (t0 // TP) % 2

            def bc(v):
                return v[:, t0:t0 + TP].unsqueeze(1).broadcast_to([128, 128, TP])
            tt('dve', Aoh[:, ob, :, :], bc(Ism), ciota[:, :, :], ALU.is_equal, reads=['Ifsm', 'ciota'], writes=[('Aoh', ob)])
            tt('dve', Aoh[:, ob, :, :], Aoh[:, ob, :, :], bc(Gsm), ALU.mult, reads=[('Aoh', ob), 'Gfsm'], writes=[('Aoh', ob)])
            tt('dve', Boh[:, ob, :, :], bc(Jsm), ciota[:, :, :], ALU.is_equal, reads=['Jfsm', 'ciota'], writes=[('Boh', ob)])
            for t4 in range(0, TP, 4):
                bank = 6 + ((t0 + t4) // 4) % 2
                for q in range(4):
                    mm(psb[bank][:, q * 128:(q + 1) * 128], Aoh[:, ob, :, t4 + q], Boh[:, ob, :, t4 + q], True, True,
                       reads=[('Aoh', ob), ('Boh', ob)], wkey=('psb', bank), inc=(q == 3))
                cp('act', Wt[:, t0 + t4:t0 + t4 + 4, :], psb[bank][:, :].rearrange('p (a b) -> p a b', a=4),
                   reads=[('psb', bank)], writes=['Wt'])
        accs = list(range(0, 8))
        hrot = Rot([8, 10, 12, 14])
        NJ = 128
        PIPE = 2

        def u_side(j):
            ju, jj = j // 8, j % 8
            su = peer_slots[ju][0]
            ru = ring[:, su, :].rearrange('p (j c i) -> p j c i', j=8, c=8)
            ph = hrot.next()
            for c in range(8):
                mm(pst(ph), ru[:, jj, c, :], hT[:, c, :], c == 0, c == 7, reads=[hkeys[c], ('ring', su)], wkey=psk(ph), inc=(c == 7))
            b4 = j % 4
            act(GJ[:, b4, :], pst(ph), AF.Gelu, reads=[psk(ph)], writes=[('GJ', b4)])
            tt('dve', AW[:, b4, :], GJ[:, b4, :], Wt[:, :, j], ALU.mult, reads=[('GJ', b4), 'Wt'], writes=[('AW', b4)])

        def v_side(j):
            ju, jj = j // 8, j % 8
            sv = peer_slots[ju][1]
            rv = ring[:, sv, :].rearrange('p (j d) -> p j d', j=8)
            b4 = j % 4
            for e in range(8):
                S.op('pe', lambda e=e: nc.tensor.matmul(pst(accs[e]), lhsT=rv[:, jj, e * 128:(e + 1) * 128], rhs=AW[:, b4, :],
                                                        start=(j == 0 and e % 2 == 0), stop=(j == NJ - 1),
                                                        skip_group_check=True),
                     reads=[('AW', b4), ('ring', sv)], writes=[psk(accs[e])], inc=(e == 7))
        for j in range(NJ + PIPE):
            if j < NJ:
                u_side(j)
            if j >= PIPE:
                jv = j - PIPE
                v_side(jv)
                if jv % 8 == 7 and jv // 8 + 2 < 16:
                    g = jv // 8 + 2
                    peer_slots[g] = (ring_load(U_PEER + 2 * g), ring_load(U_PEER + 2 * g + 1))
                elif jv % 8 == 7 and b + 1 < nblk:
                    if jv // 8 == 14 and 'attn' in phases:
                        pre[('att', b + 1)] = (ring_load(U_WQ), ring_load(U_WXO))
                    elif jv // 8 == 15:
                        pre[('pq', b + 1)] = (ring_load(U_PQ0), ring_load(U_PQ1))
        for e in range(8):
            tt('dve', xb[:, e, :], xb[:, e, :], pst(accs[e]), ALU.add, reads=[xkeys[e], psk(accs[e])], writes=[xkeys[e]])

    def final(b):
        xb = xTs[b % 2]
        xkeys = xk(b)
        M.top = DYN
        S.phase(['yo'])
        yo = M.alloc([128, 8, T], F32)
        okeys = [('yo', c) for c in range(8)]
        rmsnorm_to(xb, xkeys, PC_GFIN, yo, okeys, T, 0)
        S.dma('sp', out=outr[b], in_=yo.rearrange('p a b -> p (a b)'), reads=okeys, writes=[('out', b)])

    def drain(g):
        for _ in g:
            pass

    TK_BYTES = 25 * 1024
    load_x(0)
    S.phase(MIX_NAMES)
    drain(gen_mixer(0, DYN))
    for b in range(nblk):
        if 'attn' in phases:
            attn(b)
        if b + 1 < nblk:
            load_x(b + 1)
        if 'peer' in phases:
            peer0(b)
            S.phase(TK_NAMES + MIX_NAMES)
            g1 = gen_topk(b, DYN)
            g2 = gen_mixer(b + 1, DYN + TK_BYTES) if b + 1 < nblk else iter(())
            done1 = done2 = False
            while not (done1 and done2):
                if not done1:
                    try:
                        next(g1)
                    except StopIteration:
                        done1 = True
                if not done2:
                    try:
                        next(g2)
                    except StopIteration:
                        done2 = True
            peer_wb_uv(b)
        elif b + 1 < nblk:
            S.phase(MIX_NAMES)
            drain(gen_mixer(b + 1, DYN))
        final(b)
    S.barrier()
    es.close()
    return nc, S


def _prep_shared(inp):
    f = np.float32
    W = np.zeros((NUNITS, 128, UNIT), f)
    w_in = np.asarray(inp['w_in'][0], f)
    t = w_in.reshape(8, 128, 7, 8, 128)
    t = t.transpose(3, 1, 0, 2, 4).reshape(8, 128, 8 * 7 * 128)
    W[U_WIN:U_WIN + 8, :, :7168] = t

    def sq(w):
        w = np.asarray(w, f)
        return w.reshape(8, 128, w.shape[1]).transpose(1, 0, 2).reshape(128, -1)
    W[U_SCO] = sq(inp['sc_w_out'][0])
    W[U_PW2] = sq(inp['cf_w_pw2'][0])
    W[U_MIX] = sq(inp['w_mix_out'][0])
    W[U_WQ] = sq(inp['w_q'][0])
    W[U_WXO] = sq(inp['w_xo'][0])
    wpq = np.asarray(inp['w_peer_q'][0], f)
    W[U_PQ0] = sq(wpq[:, :1024])
    W[U_PQ1] = sq(wpq[:, 1024:])
    wkv = np.asarray(inp['w_kv'][0], f)
    W[U_WK] = sq(wkv[:, :1024])
    W[U_WV] = sq(wkv[:, 1024:])
    u = np.asarray(inp['peer_u'][0], f).reshape(128, 16, 8, 8, 128)
    u = u.transpose(1, 4, 2, 3, 0).reshape(16, 128, UNIT)
    v = np.asarray(inp['peer_v'][0], f).reshape(128, 16, 8, 1024)
    v = v.transpose(1, 0, 2, 3).reshape(16, 128, UNIT)
    W[U_PEER:U_PEER + 32:2] = u
    W[U_PEER + 1:U_PEER + 32:2] = v

    P = np.zeros((128, NPAR), f)

    def col(vv):
        return np.asarray(vv, f).reshape(8, 128).T
    P[:, PC_GMIX:PC_GMIX + 8] = col(inp['norm_mix_g'][0])
    b1 = np.asarray(inp['cf_b_pw1'][0], f)
    P[:, PC_B1A:PC_B1A + 8] = col(b1[:1024])
    P[:, PC_B1G:PC_B1G + 8] = col(b1[1024:])
    P[:, PC_CONVB:PC_CONVB + 8] = col(inp['cf_conv_b'][0])
    P[:, PC_LNG:PC_LNG + 8] = col(inp['cf_ln_g'][0])
    P[:, PC_LNB:PC_LNB + 8] = col(inp['cf_ln_b'][0])
    P[:, PC_BPW2:PC_BPW2 + 8] = col(inp['cf_b_pw2'][0])
    P[:, PC_GXA:PC_GXA + 8] = col(inp['norm_xa_g'][0])
    P[:, PC_GMEM:PC_GMEM + 8] = col(inp['norm_mem_g'][0])
    P[:, PC_GPEER:PC_GPEER + 8] = col(inp['norm_peer_g'][0])
    P[:, PC_GFIN:PC_GFIN + 8] = col(inp['final_norm_g'])
    scw = np.asarray(inp['sc_conv_w'][0], f)
    P[:, PC_SCW:PC_SCW + 24] = scw.reshape(3, 8, 128).transpose(2, 1, 0).reshape(128, 24)
    cfw = np.asarray(inp['cf_conv_w'][0], f)
    P[:, PC_CFW:PC_CFW + 248] = cfw.reshape(31, 8, 128).transpose(2, 1, 0).reshape(128, 248)
    sk = np.asarray(inp['peer_subkeys'][0], f)
    P[:, PC_SKT:PC_SKT + 256] = sk.transpose(2, 0, 1).reshape(128, 256)
    P[:, PC_IDENT:PC_IDENT + 128] = np.eye(128, dtype=f)
    P[:, PC_IOTA:PC_IOTA + 128] = np.arange(128, dtype=f)[None, :]
    return W, P


def _prep_core(x_b, mem_b):
    f = np.float32
    xb = np.asarray(x_b, f).reshape(NBLK, NB_TOK, 8, 128)
    xr = np.ascontiguousarray(xb.transpose(0, 3, 2, 1)).reshape(NBLK, 128, 8 * NB_TOK)
    mb = np.asarray(mem_b, f).reshape(NMEM, 8, 128)
    mr = np.ascontiguousarray(mb.transpose(2, 1, 0)).reshape(128, 8 * NMEM)
    return xr, mr


def _unprep_out(o):
    o = o.reshape(NBLK, 128, 8, NB_TOK)
    return np.ascontiguousarray(o.transpose(0, 3, 2, 1)).reshape(SEQ, D)


def kernel(**inputs):
    n = 8
    W, P = _prep_shared(inputs)
    nc, _ = build_nc()
    in_maps = []
    for b in range(n):
        xr, mr = _prep_core(inputs['x'][b], inputs['mem'][b])
        in_maps.append({"xr": xr, "memr": mr, "wunits": W, "params": P})
    res = run_bass_kernel_spmd(nc, in_maps, core_ids=list(range(n)))
    out = np.stack([_unprep_out(np.asarray(r["outr"])) for r in res.results], axis=0)
    return out.astype(np.float32)
```
